# Optimizing a Trainium2 kernel written in Bass

```python
import jax, jax.numpy as jnp
from jax import lax
import numpy as np

D_MODEL = 2048
BATCH = 4
SEQ = 4096
DEPTH = 2

CTX_LEN = 256
GRID_W = 64
N_MOD = 6
EPS = 1e-6

D_CONV = D_MODEL // 2
CONV_WIDTH = 3
D_POOL = D_MODEL // 2
POOL_WINDOWS = (2, 4, 8, 16)
N_POOL_GROUPS = len(POOL_WINDOWS)
POOL_GROUP_DIM = D_POOL // N_POOL_GROUPS
NA_HEADS = 8
NA_HEAD_DIM = 128
D_ATTN = NA_HEADS * NA_HEAD_DIM
WIN_ROWS = 8
WIN_COLS = 16
N_BRANCHES = 3

N_EXPERTS = 16
EXPERT_HIDDEN = 2048
CAPACITY_FACTOR = 2

OFF_CONV = 0
OFF_POOL = OFF_CONV + 3 * D_CONV
OFF_Q = OFF_POOL + D_POOL
OFF_K = OFF_Q + D_ATTN
OFF_V = OFF_K + D_ATTN
OFF_GATE = OFF_V + D_ATTN
IN_COLS = OFF_GATE + N_BRANCHES * D_MODEL

kernel_name = "hybrid_conv_pool_natten_ecmoe_prefix_trunk"


def rms_norm(x, g):
    xf = x.astype(jnp.float32)
    y = xf * lax.rsqrt(jnp.mean(xf * xf, axis=-1, keepdims=True) + EPS)
    return (y * g.astype(jnp.float32)).astype(x.dtype)


def adaln(cond, w_mod, b_mod):
    m = jax.nn.silu(cond) @ w_mod + b_mod
    return m.reshape(*cond.shape[:-1], N_MOD, D_MODEL)


def mod_chunk(m, i):
    return m[..., None, i, :]


def modulate(h, shift, scale):
    return h * (1 + scale) + shift


def short_conv(u, w):
    n = u.shape[1]
    up = jnp.pad(u, ((0, 0), (1, 1), (0, 0)))
    return up[:, 0:n] * w[0] + up[:, 1:n + 1] * w[1] + up[:, 2:n + 2] * w[2]


def multiscale_pool(u, pool_w, pool_scale):
    b, n, _ = u.shape
    ug = u.astype(jnp.float32).reshape(b, n, N_POOL_GROUPS, POOL_GROUP_DIM)
    cs = jnp.concatenate([jnp.zeros_like(ug[:, :1]), jnp.cumsum(ug, axis=1)], axis=1)
    t = jnp.arange(n)[:, None]
    win = jnp.array(POOL_WINDOWS, dtype=jnp.int32)[None, :]
    lo = jnp.clip(t - win // 2, 0, n)
    hi = jnp.clip(t - win // 2 + win, 0, n)
    g = jnp.arange(N_POOL_GROUPS)[None, :]
    count = (hi - lo).astype(jnp.float32)[None, :, :, None]
    mean = (cs[:, hi, g] - cs[:, lo, g]) / count
    mixed = (mean - ug).astype(u.dtype)
    y = jnp.einsum('bngc,gcd->bngd', mixed, pool_w).reshape(b, n, D_POOL)
    return y * pool_scale


def split_heads(proj, off):
    b, n, _ = proj.shape
    return proj[..., off:off + D_ATTN].reshape(b, n, NA_HEADS, NA_HEAD_DIM)


def context_attention(q, k, v):
    s = jnp.einsum('bqhd,bkhd->bhqk', q, k).astype(jnp.float32) * (NA_HEAD_DIM ** -0.5)
    p = jax.nn.softmax(s, axis=-1).astype(v.dtype)
    o = jnp.einsum('bhqk,bkhd->bqhd', p, v)
    return o.reshape(q.shape[0], q.shape[1], D_ATTN)


def neighbourhood_attention(q, k, v, k_ctx, v_ctx, rpb):
    b, n, h, dh = q.shape
    rows = n // GRID_W
    kr = min(WIN_ROWS, rows)
    qg = q.reshape(b, rows, GRID_W, h, dh)
    kg = k.reshape(b, rows, GRID_W, h, dh)
    vg = v.reshape(b, rows, GRID_W, h, dh)
    col = jnp.arange(GRID_W)
    col_start = jnp.clip(col - WIN_COLS // 2, 0, GRID_W - WIN_COLS)
    col_idx = col_start[:, None] + jnp.arange(WIN_COLS)[None, :]
    bias_cols = rpb[:, :, col_idx - col[:, None] + WIN_COLS - 1]
    scale = dh ** -0.5
    n_win = kr * WIN_COLS

    def row_block(r):
        rs = jnp.clip(r - kr // 2, 0, rows - kr)
        q_r = lax.dynamic_index_in_dim(qg, r, axis=1, keepdims=False)
        k_win = lax.dynamic_slice_in_dim(kg, rs, kr, axis=1)[:, :, col_idx]
        v_win = lax.dynamic_slice_in_dim(vg, rs, kr, axis=1)[:, :, col_idx]
        bias = jnp.take(bias_cols, rs + jnp.arange(kr) - r + WIN_ROWS - 1, axis=1)
        s_win = (jnp.einsum('bqhd,brqjhd->bhqrj', q_r, k_win).astype(jnp.float32) * scale
                 + jnp.transpose(bias, (0, 2, 1, 3)).astype(jnp.float32))
        s_ctx = jnp.einsum('bqhd,blhd->bhql', q_r, k_ctx).astype(jnp.float32) * scale
        s = jnp.concatenate([s_win.reshape(b, h, GRID_W, n_win), s_ctx], axis=-1)
        p = jax.nn.softmax(s, axis=-1).astype(v.dtype)
        p_win = p[..., :n_win].reshape(b, h, GRID_W, kr, WIN_COLS)
        p_ctx = p[..., n_win:]
        return (jnp.einsum('bhqrj,brqjhd->bqhd', p_win, v_win)
                + jnp.einsum('bhql,blhd->bqhd', p_ctx, v_ctx))

    out = lax.map(row_block, jnp.arange(rows))
    return jnp.moveaxis(out, 0, 1).reshape(b, n, h * dh)


def merge_branches(proj, attn, conv_w, pool_w, pool_scale, w_conv_out, w_pool_out, w_attn_out, w_o):
    b, n, _ = proj.shape
    a_b = proj[..., OFF_CONV:OFF_CONV + D_CONV]
    a_c = proj[..., OFF_CONV + D_CONV:OFF_CONV + 2 * D_CONV]
    a_x = proj[..., OFF_CONV + 2 * D_CONV:OFF_POOL]
    y_conv = (a_b * short_conv(a_c * a_x, conv_w)) @ w_conv_out
    y_pool = multiscale_pool(proj[..., OFF_POOL:OFF_Q], pool_w, pool_scale) @ w_pool_out
    y_attn = attn @ w_attn_out
    gates = jax.nn.sigmoid(proj[..., OFF_GATE:].reshape(b, n, N_BRANCHES, D_MODEL))
    merged = gates[..., 0, :] * y_conv + gates[..., 1, :] * y_pool + gates[..., 2, :] * y_attn
    return merged @ w_o


def expert_choice_ffn(h, w_router, w_gate, w_up, w_down):
    b, n, _ = h.shape
    cap = max(1, CAPACITY_FACTOR * n // N_EXPERTS)
    aff = jax.nn.softmax((h @ w_router).astype(jnp.float32), axis=-1)
    g, idx = lax.top_k(jnp.swapaxes(aff, 1, 2), cap)
    bidx = jnp.arange(b)[:, None, None]
    xs = h[bidx, idx]
    hid = (jax.nn.silu(jnp.einsum('becd,edf->becf', xs, w_gate))
           * jnp.einsum('becd,edf->becf', xs, w_up))
    ys = jnp.einsum('becf,efd->becd', hid, w_down) * g[..., None].astype(h.dtype)
    return jnp.zeros_like(h).at[bidx, idx].add(ys)


def setup_inputs(seed: int = 0) -> dict:
    key = jax.random.key(seed)
    ks = jax.random.split(key, 24)
    f32 = jnp.float32

    def nrm(k, shape, s):
        return jax.random.normal(k, shape, f32) * s

    return {
        "x": nrm(ks[0], (BATCH, SEQ, D_MODEL), 1.0),
        "c": nrm(ks[1], (BATCH, D_MODEL), 1.0),
        "ctx": nrm(ks[2], (BATCH, CTX_LEN, D_MODEL), 1.0),
        "c_ctx": nrm(ks[3], (D_MODEL,), 1.0),
        "w_mod": nrm(ks[4], (DEPTH, D_MODEL, N_MOD * D_MODEL), 0.5 * D_MODEL ** -0.5),
        "b_mod": nrm(ks[5], (DEPTH, N_MOD * D_MODEL), 0.02),
        "norm1": 1.0 + nrm(ks[6], (DEPTH, D_MODEL), 0.05),
        "norm2": 1.0 + nrm(ks[7], (DEPTH, D_MODEL), 0.05),
        "w_in": nrm(ks[8], (DEPTH, D_MODEL, IN_COLS), D_MODEL ** -0.5),
        "conv_w": nrm(ks[9], (DEPTH, CONV_WIDTH, D_CONV), CONV_WIDTH ** -0.5),
        "pool_w": nrm(ks[10], (DEPTH, N_POOL_GROUPS, POOL_GROUP_DIM, POOL_GROUP_DIM), POOL_GROUP_DIM ** -0.5),
        "pool_scale": 1.0 + nrm(ks[11], (DEPTH, D_POOL), 0.05),
        "rpb": nrm(ks[12], (DEPTH, NA_HEADS, 2 * WIN_ROWS - 1, 2 * WIN_COLS - 1), 0.1),
        "w_conv_out": nrm(ks[13], (DEPTH, D_CONV, D_MODEL), D_CONV ** -0.5),
        "w_pool_out": nrm(ks[14], (DEPTH, D_POOL, D_MODEL), D_POOL ** -0.5),
        "w_attn_out": nrm(ks[15], (DEPTH, D_ATTN, D_MODEL), D_ATTN ** -0.5),
        "w_o": nrm(ks[16], (DEPTH, D_MODEL, D_MODEL), D_MODEL ** -0.5),
        "w_router": nrm(ks[17], (DEPTH, D_MODEL, N_EXPERTS), D_MODEL ** -0.5),
        "w_e_gate": nrm(ks[18], (DEPTH, N_EXPERTS, D_MODEL, EXPERT_HIDDEN), D_MODEL ** -0.5),
        "w_e_up": nrm(ks[19], (DEPTH, N_EXPERTS, D_MODEL, EXPERT_HIDDEN), D_MODEL ** -0.5),
        "w_e_down": nrm(ks[20], (DEPTH, N_EXPERTS, EXPERT_HIDDEN, D_MODEL), EXPERT_HIDDEN ** -0.5),
        "final_norm": 1.0 + nrm(ks[21], (D_MODEL,), 0.05),
    }


def reference(x, c, ctx, c_ctx, w_mod, b_mod, norm1, norm2, w_in, conv_w, pool_w, pool_scale, rpb,
              w_conv_out, w_pool_out, w_attn_out, w_o, w_router, w_e_gate, w_e_up, w_e_down, final_norm):
    for l in range(DEPTH):
        last = l == DEPTH - 1
        mod_x = adaln(c, w_mod[l], b_mod[l])
        mod_c = adaln(c_ctx, w_mod[l], b_mod[l])

        hc = modulate(rms_norm(ctx, norm1[l]), mod_chunk(mod_c, 0), mod_chunk(mod_c, 1))
        if last:
            kv_c = hc @ w_in[l][:, OFF_K:OFF_GATE]
            k_ctx = split_heads(kv_c, 0)
            v_ctx = split_heads(kv_c, D_ATTN)
        else:
            pc = hc @ w_in[l]
            k_ctx = split_heads(pc, OFF_K)
            v_ctx = split_heads(pc, OFF_V)
            attn_c = context_attention(split_heads(pc, OFF_Q), k_ctx, v_ctx)
            ctx_next = ctx + mod_chunk(mod_c, 2) * merge_branches(
                pc, attn_c, conv_w[l], pool_w[l], pool_scale[l],
                w_conv_out[l], w_pool_out[l], w_attn_out[l], w_o[l])
            hc2 = modulate(rms_norm(ctx_next, norm2[l]), mod_chunk(mod_c, 3), mod_chunk(mod_c, 4))
            ctx_next = ctx_next + mod_chunk(mod_c, 5) * expert_choice_ffn(
                hc2, w_router[l], w_e_gate[l], w_e_up[l], w_e_down[l])

        hx = modulate(rms_norm(x, norm1[l]), mod_chunk(mod_x, 0), mod_chunk(mod_x, 1))
        px = hx @ w_in[l]
        attn_x = neighbourhood_attention(split_heads(px, OFF_Q), split_heads(px, OFF_K),
                                         split_heads(px, OFF_V), k_ctx, v_ctx, rpb[l])
        x = x + mod_chunk(mod_x, 2) * merge_branches(
            px, attn_x, conv_w[l], pool_w[l], pool_scale[l],
            w_conv_out[l], w_pool_out[l], w_attn_out[l], w_o[l])
        hx2 = modulate(rms_norm(x, norm2[l]), mod_chunk(mod_x, 3), mod_chunk(mod_x, 4))
        x = x + mod_chunk(mod_x, 5) * expert_choice_ffn(
            hx2, w_router[l], w_e_gate[l], w_e_up[l], w_e_down[l])

        if not last:
            ctx = ctx_next
    return rms_norm(x, final_norm)
```

```python
import numpy as np
from contextlib import ExitStack
import concourse.bass as bass
import concourse.mybir as mybir
from concourse.bass_utils import run_bass_kernel_spmd

F32 = mybir.dt.float32
BF16 = mybir.dt.bfloat16
I32 = mybir.dt.int32
AF = mybir.ActivationFunctionType
ALU = mybir.AluOpType
AX = mybir.AxisListType

D = 2048
KC = 16
T = 4096
L = 256
NT = T + L
NTILE = NT // 128
GW = 64
ROWS = T // GW
NE = 16
CAPX = 512
CAPC = 32
NSLOT = CAPX + CAPC
IN_COLS = 13312
NCH = IN_COLS // 128
CH_AB, CH_AC, CH_AX, CH_POOL, CH_Q, CH_K, CH_V, CH_G = 0, 8, 16, 24, 32, 40, 48, 56
EPS = 1e-6
BIG = 100000.0
NEG = -30000.0
DEPTH = 2
NCORES = 4


class Buf:
    __slots__ = ('w', 'r')

    def __init__(self):
        self.w = {}
        self.r = {}


class Sched:
    ENG = ('pe', 'act', 'dve', 'pool', 'sp')
    ROT = 30000

    def __init__(self, nc, stack, n_dma=64):
        self.nc = nc
        self.stack = stack
        self.ops = {e: [] for e in self.ENG}
        self.handles = []
        self.cur = {}
        self.cnt = {}
        self.mine = {e: set() for e in self.ENG}
        self.waited = {e: {} for e in self.ENG}
        self.final = {}
        self.spare = {e: [self._alloc(f"e_{e}_{i}") for i in range(6 if e == 'pe' else 2)] for e in self.ENG}
        for e in self.ENG:
            self._new_eng_sem(e)
        self.dkeys = []
        self.dcnt = []
        for i in range(n_dma):
            self.dkeys.append(self._alloc(f"dq{i}"))
            self.dcnt.append(0)
        self.drr = 0
        self.ninst = 0

    def _alloc(self, name):
        h = self.stack.enter_context(self.nc.semaphore(name))
        self.handles.append(h)
        return len(self.handles) - 1

    def _new_eng_sem(self, e):
        if e in self.cur:
            self.final[self.cur[e]] = self.cnt[e]
        k = self.spare[e].pop(0)
        self.cur[e] = k
        self.cnt[e] = 0
        self.mine[e].add(k)

    def _wait(self, e, k, v):
        w = self.waited[e]
        if w.get(k, 0) >= v:
            return
        w[k] = v
        self.ops[e].append(('w', k, v))

    def _deps(self, e, reads, writes):
        deps = {}
        for b in reads:
            for k, v in b.w.items():
                if deps.get(k, 0) < v:
                    deps[k] = v
        for b in writes:
            for d in (b.w, b.r):
                for k, v in d.items():
                    if deps.get(k, 0) < v:
                        deps[k] = v
        for k, v in deps.items():
            if e == 'pe' and k in self.mine[e]:
                continue
            self._wait(e, k, v)

    def _mark(self, k, v, reads, writes):
        for b in reads:
            if b.r.get(k, 0) < v:
                b.r[k] = v
        for b in writes:
            b.w = {k: v}
            b.r = {}

    def op(self, e, fn, reads=(), writes=()):
        self._deps(e, reads, writes)
        if self.cnt[e] >= self.ROT:
            self._new_eng_sem(e)
        self.cnt[e] += 1
        k = self.cur[e]
        self.ops[e].append(('i', fn, k))
        self._mark(k, self.cnt[e], reads, writes)
        self.ninst += 1

    def dma(self, q, fn, reads=(), writes=()):
        self._deps(q, reads, writes)
        i = self.drr
        self.drr = (self.drr + 1) % len(self.dkeys)
        k = self.dkeys[i]
        prev = self.dcnt[i]
        if prev > 0:
            self._wait(q, k, prev)
        self.dcnt[i] = prev + 16
        self.ops[q].append(('d', fn, k))
        self._mark(k, prev + 16, reads, writes)
        self.ninst += 1

    def barrier(self):
        for e in self.ENG:
            for i, k in enumerate(self.dkeys):
                if self.dcnt[i] > 0:
                    self._wait(e, k, self.dcnt[i])
            for e2 in self.ENG:
                if e2 != e and self.cnt[e2] > 0:
                    self._wait(e, self.cur[e2], self.cnt[e2])
                for k, v in self.final.items():
                    if k in self.mine[e2] and e2 != e:
                        self._wait(e, k, v)

    def getreg(self, eng, val):
        if val not in self.regs:
            self.regs[val] = eng.to_reg(val)
        return self.regs[val]

    def flush(self):
        self.regs = {}
        ops = self.ops
        self.ops = {e: [] for e in self.ENG}
        H = self.handles

        def replay(lst):
            def run(eng):
                for o in lst:
                    if o[0] == 'w':
                        eng.wait_ge(H[o[1]], o[2])
                    elif o[0] == 'i':
                        o[1](eng).then_inc(H[o[2]], 1)
                    else:
                        o[1](eng).then_inc(H[o[2]], 16)
            return run
        self.nflush = getattr(self, 'nflush', 0) + 1
        with self.nc.named_scope(f"ph{self.nflush:02d}"):
            with self.nc.Block() as block:
                block.sync(replay(ops['sp']))
                block.tensor(replay(ops['pe']))
                block.scalar(replay(ops['act']))
                block.vector(replay(ops['dve']))
                block.gpsimd(replay(ops['pool']))


class Ring:
    def __init__(self, tiles):
        self.t = [(t, Buf()) for t in tiles]
        self.i = 0

    def next(self):
        r = self.t[self.i]
        self.i = (self.i + 1) % len(self.t)
        return r


def build(cfg):
    nc = bass.Bass("TRN2", target_bir_lowering=False)
    layers = cfg.get('layers', list(range(DEPTH)))
    stop_after = cfg.get('stop_after', None)
    dbg = cfg.get('dbg', [])

    order = ['P0', 'P1', 'P2', 'P4', 'P5', 'P6', 'P7', 'P8', 'P9', 'P10', 'P11']
    needs = {'P0': ['cT', 'w_mod', 'b_modT', 'norm1T'], 'P1': ['x', 'ctx'], 'P2': ['w_in'],
             'P4': ['conv_wT', 'pool_w', 'pool_scaleT', 'rc_tab'], 'P5': ['bias_tab'],
             'P6': ['w_conv_out', 'w_pool_out', 'w_attn_out', 'w_o'], 'P7': ['norm2', 'w_router'],
             'P8': [], 'P9': [], 'P10': ['w_e_gate', 'w_e_up', 'w_e_down'], 'P11': ['final_norm']}
    if stop_after is not None and stop_after[0] == 0 and layers == [0]:
        needed = set()
        for ph in order[:order.index(stop_after[1]) + 1]:
            needed.update(needs[ph])
    else:
        needed = set(sum(needs.values(), []))
    declared = []

    def din(name, shape, dt=F32):
        if name not in needed:
            return None
        declared.append(name)
        return nc.dram_tensor(name, shape, dt, kind="ExternalInput").ap()

    x_in = din("x", [T, D])
    ctx_in = din("ctx", [L, D])
    cT_in = din("cT", [128, 2 * KC])
    w_mod = din("w_mod", [DEPTH, D, 6 * D])
    b_modT = din("b_modT", [DEPTH, 128, 96])
    norm1T = din("norm1T", [DEPTH, 128, KC])
    norm2row = din("norm2", [DEPTH, 1, D])
    w_in = din("w_in", [DEPTH, D, IN_COLS])
    conv_wT = din("conv_wT", [DEPTH, 128, 8 * 3])
    pool_w = din("pool_w", [DEPTH, 4, 256, 256])
    pool_scaleT = din("pool_scaleT", [DEPTH, 128, 8])
    rc_tab = din("rc_tab", [4, NT])
    bias_tab = din("bias_tab", [DEPTH, 128, 4 * 15 * 64], BF16)
    w_conv_out = din("w_conv_out", [DEPTH, 1024, D])
    w_pool_out = din("w_pool_out", [DEPTH, 1024, D])
    w_attn_out = din("w_attn_out", [DEPTH, 1024, D])
    w_o = din("w_o", [DEPTH, D, D])
    w_router = din("w_router", [DEPTH, D, NE])
    w_e_gate = din("w_e_gate", [DEPTH, NE, D, D])
    w_e_up = din("w_e_up", [DEPTH, NE, D, D])
    w_e_down = din("w_e_down", [DEPTH, NE, D, D])
    fnorm_row = din("final_norm", [1, D])
    out = nc.dram_tensor("out", [T, D], F32, kind="ExternalOutput").ap()

    def scr(name, shape, dt):
        kind = "ExternalOutput" if name in dbg else "Internal"
        return nc.dram_tensor(name, shape, dt, kind=kind).ap()

    HT = scr("HT", [KC, 128, NT], BF16)
    PT = scr("PT", [NCH, 128, NT], BF16)
    VT = scr("VT", [NT, 1024], BF16)
    CT = scr("CT", [8, 128, NT], BF16)
    PO = scr("PO", [8, 128, NT], BF16)
    AT = scr("AT", [8, 128, NT], BF16)
    MT = scr("MT", [KC, 128, NT], BF16)
    XA = scr("XA", [NT, D], F32)
    XB = scr("XB", [NT, D], F32)
    H2 = scr("H2", [NT, D], BF16)
    XS = [scr(f"XS{e}", [NSLOT, D], BF16) for e in range(NE)]
    YS = [scr(f"YS{e}", [NSLOT, D], F32) for e in range(NE)]
    MODROW = scr("MODROW", [DEPTH, 2, 96 * 128], F32)
    AFFD = scr("AFFD", [NT, NE], F32)
    AFFT = scr("AFFT", [NE, NT], F32)
    IDXD = scr("IDXD", [NT, NE], F32)

    with ExitStack() as top:
        S = Sched(nc, top)

        uid = [0]

        def sbt(st, name, shape, dt):
            uid[0] += 1
            return st.enter_context(nc.sbuf_tensor(f"s{uid[0]}_{name}", shape, dt))

        def pst(st, name, shape, dt):
            uid[0] += 1
            return st.enter_context(nc.psum_tensor(f"p{uid[0]}_{name}", shape, dt))

        def MM(o, lhsT, rhs, start, stop, r, w):
            S.op('pe', lambda e: e.matmul(o, lhsT=lhsT, rhs=rhs, start=start, stop=stop), r, w)

        def TR(o, i, ident, r, w):
            S.op('pe', lambda e: e.transpose(o, i, ident), r, w)

        def ACT(o, i, func, r, w, bias=None, scale=None, accum=None):
            kw = {}
            if bias is not None:
                kw['bias'] = bias
            if scale is not None:
                kw['scale'] = scale
            if accum is not None:
                kw['accum_out'] = accum
            S.op('act', lambda e: e.activation(out=o, in_=i, func=func, **kw), r, w)

        def TT(eng, o, a, b, op, r, w):
            S.op(eng, lambda e: e.tensor_tensor(out=o, in0=a, in1=b, op=op), r, w)

        def TS(eng, o, a, s1, s2, op0, op1, r, w, accum=None):
            if op1 is None:
                S.op(eng, lambda e: e.tensor_scalar(out=o, in0=a, scalar1=s1, scalar2=None, op0=op0), r, w)
            elif accum is None:
                S.op(eng, lambda e: e.tensor_scalar(out=o, in0=a, scalar1=s1, scalar2=s2, op0=op0, op1=op1), r, w)
            else:
                S.op(eng, lambda e: e.tensor_scalar(out=o, in0=a, scalar1=s1, scalar2=s2, op0=op0, op1=op1, accum_out=accum), r, w)

        def STT(eng, o, a, s, b, op0, op1, r, w):
            S.op(eng, lambda e: e.scalar_tensor_tensor(out=o, in0=a, scalar=s, in1=b, op0=op0, op1=op1), r, w)

        def CP(eng, o, i, r, w):
            S.op(eng, lambda e: e.tensor_copy(out=o, in_=i), r, w)

        def MS(eng, o, val, w):
            S.op(eng, lambda e: e.memset(o, val), (), w)

        def DMA(q, o, i, r=(), w=()):
            S.dma(q, lambda e: e.dma_start(out=o, in_=i), r, w)

        def phase_end():
            S.barrier()
            S.flush()

        def pipeline(items, load, comp, ahead=1):
            loaded = []
            n = len(items)
            for i in range(min(ahead, n)):
                loaded.append(load(items[i]))
            for i in range(n):
                if i + ahead < n:
                    loaded.append(load(items[i + ahead]))
                comp(items[i], loaded[i])
                loaded[i] = None

        def CDMA(o, i, w):
            S.dma('pool', lambda e: e.dma_start(out=o, in_=i), (), w)

        identb = sbt(top, "identb", [128, 128], BF16)
        identf = sbt(top, "identf", [128, 128], F32)
        modT = sbt(top, "modT", [128, 96, 2], F32)
        A1 = sbt(top, "A1", [128, KC, 2], F32)
        idxi = sbt(top, "idxi", [128, NTILE, NE], I32)
        gmt = sbt(top, "gmt", [128, NTILE, NE], F32)
        B_id, B_mod, B_rt = Buf(), Buf(), Buf()
        MS('pool', identf[:], 0.0, [B_id])
        S.op('pool', lambda e: e.affine_select(out=identf[:], in_=identf[:], pattern=[[-1, 128]],
                                               compare_op=ALU.not_equal, fill=1.0, base=0, channel_multiplier=1),
             [B_id], [B_id])
        CP('dve', identb[:], identf[:], [B_id], [B_id])
        phase_end()

        def src_rows(l, t0, n):
            if l == 0:
                if t0 < L:
                    return ctx_in[t0:t0 + n, :]
                return x_in[t0 - L:t0 - L + n, :]
            return XB[t0:t0 + n, :]

        def done(l, ph):
            return stop_after is not None and (l, ph) == tuple(stop_after)

        def blocks(l_last):
            bl = [(L + i * 512, 512) for i in range(8)]
            return bl if l_last else [(0, L)] + bl

        finished = False
        for l in layers:
            if finished:
                break
            last = (l == DEPTH - 1)
            tiles_all = list(range(NTILE))
            tiles_x = list(range(2, NTILE))
            tiles_moe = tiles_x if last else tiles_all
            nslot = CAPX if last else NSLOT

            with ExitStack() as st:
                cT = sbt(st, "cT", [128, 2 * KC], F32)
                scT = sbt(st, "scT", [128, 2 * KC], F32)
                bmT = sbt(st, "bmT", [128, 96], F32)
                wts = Ring([sbt(st, f"wm{i}", [128, KC, 512], F32) for i in range(2)])
                pss = Ring([pst(st, f"pm{i}", [128, 2], F32) for i in range(4)])
                B_c, B_bm = Buf(), Buf()
                DMA('sp', cT[:], cT_in, (), [B_c])
                DMA('sp', bmT[:], b_modT[l], (), [B_bm])
                ACT(scT[:], cT[:], AF.Silu, [B_c], [B_c])
                wv = w_mod[l].rearrange("(kc p) c -> p kc c", p=128)
                def p0_load(cb):
                    wt, Bw = wts.next()
                    DMA('sp', wt[:], wv[:, :, cb * 512:(cb + 1) * 512], (), [Bw])
                    return wt, Bw

                def p0_comp(cb, ld):
                    wt, Bw = ld
                    for j in range(4):
                        ps, Bp = pss.next()
                        for kc in range(KC):
                            MM(ps[:], wt[:, kc, j * 128:(j + 1) * 128], scT[:, kc * 2:(kc + 1) * 2],
                               kc == 0, kc == KC - 1, [Bw, B_c], [Bp])
                        ch = cb * 4 + j
                        TS('dve', modT[:, ch, :], ps[:], bmT[:, ch:ch + 1], None, ALU.add, None, [Bp, B_bm], [B_mod])
                pipeline(list(range(24)), p0_load, p0_comp)
                pr = pst(st, "pr", [96, 2, 128], F32)
                rows = sbt(st, "rows", [96, 2, 128], F32)
                B_pr, B_rows = Buf(), Buf()
                mflat = sbt(st, "mflat", [128, 2, 96], F32)
                B_mf = Buf()
                for j in range(2):
                    CP('dve', mflat[:, j, :], modT[:, :, j], [B_mod], [B_mf])
                for j in range(2):
                    TR(pr[:, j, :], mflat[:, j, :], identf[:], [B_mf, B_id], [B_pr])
                CP('dve', rows[:], pr[:], [B_pr], [B_rows])
                for j in range(2):
                    DMA('sp', MODROW[l, j].rearrange("(c p) -> c p", p=128), rows[:, j, :], [B_rows], ())
                n1 = sbt(st, "n1", [128, KC], F32)
                B_n1 = Buf()
                DMA('sp', n1[:], norm1T[l], (), [B_n1])
                for j in range(2):
                    TS('dve', A1[:, :, j], modT[:, 16:32, j], 1.0, None, ALU.add, None, [B_mod], [B_mod])
                    TT('dve', A1[:, :, j], A1[:, :, j], n1[:], ALU.mult, [B_mod, B_n1], [B_mod])
                phase_end()
            if done(l, 'P0'):
                finished = True
                break

            with ExitStack() as st:
                xts = Ring([sbt(st, f"xt{i}", [128, D], F32) for i in range(3)])
                junk = sbt(st, "junk", [128, D], BF16)
                xns = Ring([sbt(st, f"xn{i}", [128, D], F32) for i in range(2)])
                sss = Ring([sbt(st, f"ss{i}", [128, 2], F32) for i in range(2)])
                pts = Ring([pst(st, f"pt{i}", [128, KC, 128], F32) for i in range(2)])
                hbs = Ring([sbt(st, f"hb{i}", [128, KC, 512], BF16) for i in range(2)])
                B_junk = Buf()
                items = [(t0, n, sub) for (t0, n) in blocks(False) for sub in range(n // 128)]
                state = {}

                def p1_load(it):
                    t0, n, sub = it
                    xt, Bx = xts.next()
                    DMA('sp', xt[:], src_rows(l, t0 + sub * 128, 128), (), [Bx])
                    return xt, Bx

                def p1_comp(it, ld):
                    t0, n, sub = it
                    xt, Bx = ld
                    if sub == 0:
                        state['hb'] = hbs.next()
                    hb, Bh = state['hb']
                    xn, Bn = xns.next()
                    ss, Bs = sss.next()
                    pt, Bp = pts.next()
                    j = 1 if t0 < L else 0
                    ACT(junk[:], xt[:], AF.Square, [Bx], [B_junk, Bs], accum=ss[:, 0:1])
                    ACT(ss[:, 1:2], ss[:, 0:1], AF.Sqrt, [Bs], [Bs], bias=EPS, scale=1.0 / D)
                    S.op('dve', lambda e: e.reciprocal(out=ss[:, 1:2], in_=ss[:, 1:2]), [Bs], [Bs])
                    TS('dve', xn[:], xt[:], ss[:, 1:2], None, ALU.mult, None, [Bx, Bs], [Bn])
                    for fc in range(KC):
                        TR(pt[:, fc, :], xn[:, fc * 128:(fc + 1) * 128], identf[:], [Bn, B_id], [Bp])
                    for fc in range(KC):
                        o = hb[:, fc, sub * 128:(sub + 1) * 128]
                        if fc % 2 == 0:
                            ACT(o, pt[:, fc, :], AF.Identity, [Bp, B_mod], [Bh],
                                bias=modT[:, fc, j:j + 1], scale=A1[:, fc, j:j + 1])
                        else:
                            TS('dve', o, pt[:, fc, :], A1[:, fc, j:j + 1], modT[:, fc, j:j + 1], ALU.mult, ALU.add,
                               [Bp, B_mod], [Bh])
                    if sub == n // 128 - 1:
                        DMA('sp', HT[:, :, t0:t0 + n].rearrange("c p t -> p c t"), hb[:, :, 0:n], [Bh], ())
                pipeline(items, p1_load, p1_comp, ahead=2)
                phase_end()
            if done(l, 'P1'):
                finished = True
                break

            with ExitStack() as st:
                wts = Ring([sbt(st, f"wi{i}", [128, KC, 512], BF16) for i in range(2)])
                hbs = Ring([sbt(st, f"hb{i}", [128, KC, 512], BF16) for i in range(3)])
                obs = Ring([sbt(st, f"ob{i}", [128, 4, 512], BF16) for i in range(3)])
                pss = Ring([pst(st, f"pp{i}", [128, 512], F32) for i in range(6)])
                wv = w_in[l].rearrange("(kc p) c -> p kc c", p=128)
                NG = NCH // 4
                bl = blocks(False)
                items = [(g, bi) for g in range(NG) for bi in range(len(bl))]
                wslots = {}

                def p2_wload(g):
                    wt, Bw = wts.next()
                    CDMA(wt[:], wv[:, :, g * 512:(g + 1) * 512], [Bw])
                    wslots[g] = (wt, Bw)

                def p2_load(it):
                    g, bi = it
                    t0, n = bl[bi]
                    hb, Bh = hbs.next()
                    DMA('sp', hb[:, :, 0:n], HT[:, :, t0:t0 + n].rearrange("c p t -> p c t"), (), [Bh])
                    return hb, Bh

                def p2_comp(it, ld):
                    g, bi = it
                    t0, n = bl[bi]
                    hb, Bh = ld
                    if bi == 0:
                        if g == 0:
                            p2_wload(0)
                        if g + 1 < NG:
                            p2_wload(g + 1)
                    wt, Bw = wslots[g]
                    is_v = (CH_V <= g * 4 < CH_V + 8)
                    ob, Bo = obs.next()
                    if not is_v:
                        for j in range(4):
                            ch = g * 4 + j
                            ps, Bp = pss.next()
                            for kc in range(KC):
                                MM(ps[:, 0:n], wt[:, kc, j * 128:(j + 1) * 128], hb[:, kc, 0:n], kc == 0, kc == KC - 1,
                                   [Bw, Bh], [Bp])
                            if ch >= CH_G:
                                ACT(ob[:, j, 0:n], ps[:, 0:n], AF.Sigmoid, [Bp], [Bo])
                            elif CH_Q <= ch < CH_K:
                                TS('dve', ob[:, j, 0:n], ps[:, 0:n], 128.0 ** -0.5, None, ALU.mult, None, [Bp], [Bo])
                            elif j % 2 == 0:
                                CP('dve', ob[:, j, 0:n], ps[:, 0:n], [Bp], [Bo])
                            else:
                                ACT(ob[:, j, 0:n], ps[:, 0:n], AF.Copy, [Bp], [Bo])
                        DMA('sp', PT[g * 4:(g + 1) * 4, :, t0:t0 + n].rearrange("c p t -> p c t"), ob[:, :, 0:n], [Bo], ())
                    else:
                        vc0 = (g * 4 - CH_V) * 128
                        for sub in range(n // 128):
                            ps, Bp = pss.next()
                            for kc in range(KC):
                                MM(ps[:], hb[:, kc, sub * 128:(sub + 1) * 128], wt[:, kc, :], kc == 0, kc == KC - 1,
                                   [Bw, Bh], [Bp])
                            if sub % 2 == 0:
                                CP('dve', ob[:, sub, :], ps[:], [Bp], [Bo])
                            else:
                                ACT(ob[:, sub, :], ps[:], AF.Copy, [Bp], [Bo])
                        DMA('sp', VT[t0:t0 + n, vc0:vc0 + 512].rearrange("(s p) c -> p s c", p=128),
                            ob[:, 0:n // 128, :], [Bo], ())
                pipeline(items, p2_load, p2_comp, ahead=2)
                phase_end()
            if done(l, 'P2'):
                finished = True
                break

            segs = [(0, L), (L, T)]
            with ExitStack() as st:
                cw = sbt(st, "cw", [128, 8, 3], F32)
                B_cw = Buf()
                DMA('sp', cw[:], conv_wT[l].rearrange("p (c k) -> p c k", k=3), (), [B_cw])
                ins = Ring([sbt(st, f"ci{i}", [128, 3, NT], BF16) for i in range(2)])
                us = Ring([sbt(st, f"cu{i}", [128, NT], F32) for i in range(2)])
                vs = Ring([sbt(st, f"cv{i}", [128, NT], F32) for i in range(2)])
                ys = Ring([sbt(st, f"cy{i}", [128, NT], BF16) for i in range(2)])
                for c in range(8):
                    ci, Bi = ins.next()
                    u, Bu = us.next()
                    v, Bv = vs.next()
                    y, By = ys.next()
                    for k, ch in enumerate((CH_AB + c, CH_AC + c, CH_AX + c)):
                        DMA('sp', ci[:, k, :], PT[ch], (), [Bi])
                    eng = 'dve'
                    TT(eng, u[:], ci[:, 1, :], ci[:, 2, :], ALU.mult, [Bi], [Bu])
                    TS(eng, v[:], u[:], cw[:, c, 1:2], None, ALU.mult, None, [Bu, B_cw], [Bv])
                    for (s0, n) in segs:
                        STT(eng, v[:, s0 + 1:s0 + n], u[:, s0:s0 + n - 1], cw[:, c, 0:1], v[:, s0 + 1:s0 + n],
                            ALU.mult, ALU.add, [Bu, B_cw, Bv], [Bv])
                        STT(eng, v[:, s0:s0 + n - 1], u[:, s0 + 1:s0 + n], cw[:, c, 2:3], v[:, s0:s0 + n - 1],
                            ALU.mult, ALU.add, [Bu, B_cw, Bv], [Bv])
                    TT(eng, y[:], ci[:, 0, :], v[:], ALU.mult, [Bi, Bv], [By])
                    DMA('sp', CT[c], y[:], [By], ())
                phase_end()

            with ExitStack() as st:
                PADN = NT + 64
                offs = [16, 16 + L + 32]
                pws = Ring([sbt(st, f"pw{i}", [128, 2, 256], BF16) for i in range(2)])
                psc = sbt(st, "psc", [128, 8], F32)
                B_psc = Buf()
                DMA('sp', psc[:], pool_scaleT[l], (), [B_psc])
                uin = Ring([sbt(st, f"pu{i}", [128, NT], BF16) for i in range(2)])
                rcs = Ring([sbt(st, f"rc{i}", [128, NT], F32) for i in range(2)])
                mixed = sbt(st, "mixed", [128, 2, NT], BF16)
                B_mixed = Buf()
                bufa = sbt(st, "pba", [128, PADN], F32)
                bufb = sbt(st, "pbb", [128, PADN], F32)
                Ba_, Bb_ = Buf(), Buf()
                pob = Ring([sbt(st, f"po{i}", [128, 512], BF16) for i in range(3)])
                pss = Ring([pst(st, f"pq{i}", [128, 512], F32) for i in range(4)])
                for g in range(4):
                    pw, Bpw = pws.next()
                    S.dma('pool', (lambda o, i: (lambda e: e.dma_start(out=o, in_=i)))(
                        pw[:], pool_w[l, g].rearrange("(kc p) d -> p kc d", p=128)), (), [Bpw])
                    rc, Brc = rcs.next()
                    DMA('sp', rc[:], rc_tab[g:g + 1, :].to_broadcast([128, NT]), (), [Brc])
                    for cc in range(2):
                        c = 2 * g + cc
                        u, Bu = uin.next()
                        DMA('sp', u[:], PT[CH_POOL + c], (), [Bu])
                        MS('pool', bufa[:], 0.0, [Ba_])
                        for si, (s0, n) in enumerate(segs):
                            CP('pool', bufa[:, offs[si]:offs[si] + n], u[:, s0:s0 + n], [Bu], [Ba_])
                        cur, Bcur, oth, Both = bufa, Ba_, bufb, Bb_
                        sh = 0
                        for lev in range(g + 1):
                            if lev == 0:
                                lo, hi = 1, PADN
                                TT('dve', oth[:, lo:hi], cur[:, lo - 1:hi - 1], cur[:, lo:hi], ALU.add, [Bcur], [Both])
                            else:
                                h = 1 << (lev - 1)
                                lo, hi = 8, PADN - 8
                                TT('dve', oth[:, lo:hi], cur[:, lo - h:hi - h], cur[:, lo + h:hi + h], ALU.add, [Bcur], [Both])
                            cur, Bcur, oth, Both = oth, Both, cur, Bcur
                        for si, (s0, n) in enumerate(segs):
                            TT('dve', oth[:, offs[si]:offs[si] + n], cur[:, offs[si]:offs[si] + n], rc[:, s0:s0 + n], ALU.mult,
                               [Bcur, Brc], [Both])
                            TT('dve', mixed[:, cc, s0:s0 + n], oth[:, offs[si]:offs[si] + n], u[:, s0:s0 + n], ALU.subtract,
                               [Both, Bu], [B_mixed])
                    for dc in range(2):
                        c = 2 * g + dc
                        for (t0, n) in blocks(False):
                            ps, Bp = pss.next()
                            ob, Bo = pob.next()
                            for kc in range(2):
                                MM(ps[:, 0:n], pw[:, kc, dc * 128:(dc + 1) * 128], mixed[:, kc, t0:t0 + n], kc == 0, kc == 1,
                                   [Bpw, B_mixed], [Bp])
                            TS('dve', ob[:, 0:n], ps[:, 0:n], psc[:, c:c + 1], None, ALU.mult, None, [Bp, B_psc], [Bo])
                            DMA('sp', PO[c, :, t0:t0 + n], ob[:, 0:n], [Bo], ())
                phase_end()
            if done(l, 'P4'):
                finished = True
                break

            with ExitStack() as st:
                kcT = sbt(st, "kcT", [128, 8, L], BF16)
                vcx = sbt(st, "vcx", [128, 2, 1024], BF16)
                tb = sbt(st, "tb", [128, 4, 15 * 64], BF16)
                B_kc, B_vc, B_tb = Buf(), Buf(), Buf()
                DMA('sp', kcT[:], PT[CH_K:CH_K + 8, :, 0:L].rearrange("c p t -> p c t"), (), [B_kc])
                DMA('sp', vcx[:], VT[0:L, :].rearrange("(s p) c -> p s c", p=128), (), [B_vc])
                DMA('sp', tb[:], bias_tab[l].rearrange("q (h x) -> q h x", h=4), (), [B_tb])
                qts = Ring([sbt(st, f"qt{i}", [128, 8, 512], BF16) for i in range(2)])
                kws = Ring([sbt(st, f"kw{i}", [128, 8, 512], BF16) for i in range(3)])
                vws = Ring([sbt(st, f"vw{i}", [128, 4, 1024], BF16) for i in range(3)])
                aos = Ring([sbt(st, f"ao{i}", [128, 8, 512], BF16) for i in range(2)])
                sws = Ring([sbt(st, f"sw{i}", [128, 768], F32) for i in range(2)])
                pes = Ring([sbt(st, f"pe{i}", [128, 768], F32) for i in range(2)])
                pns = Ring([sbt(st, f"pn{i}", [128, 768], BF16) for i in range(2)])
                sts = Ring([sbt(st, f"st{i}", [128, 4], F32) for i in range(3)])
                ptsb = Ring([sbt(st, f"pts{i}", [128, 6, 128], BF16) for i in range(2)])
                ps_s = Ring([pst(st, f"pss{i}", [128, 512], F32) for i in range(2)])
                ps_c = Ring([pst(st, f"psc{i}", [128, 512], F32) for i in range(2)])
                ps_t = Ring([pst(st, f"pst{i}", [128, 6, 128], BF16) for i in range(2)])
                ps_o = Ring([pst(st, f"pso{i}", [128, 2, 64], F32) for i in range(2)])

                def attn_unit(q2, Bq, k2, Bk, kc2, v_fn, Bv, nwin, bias_ap, o_ap, Bo):
                    nk = nwin + L
                    sw, Bsw = sws.next()
                    pe_, Bpe = pes.next()
                    pn, Bpn = pns.next()
                    stt, Bst = sts.next()
                    pss_, Bpss = ps_s.next()
                    psc_, Bpsc = ps_c.next()
                    pst_, Bpst = ps_t.next()
                    pso_, Bpso = ps_o.next()
                    ptb, Bptb = ptsb.next()
                    for hh in range(2):
                        if nwin:
                            MM(pss_[hh * 64:(hh + 1) * 64, 0:nwin], q2(hh), k2(hh), True, True, [Bq, Bk], [Bpss])
                        MM(psc_[hh * 64:(hh + 1) * 64, 0:L], q2(hh), kc2(hh), True, True, [Bq, B_kc], [Bpsc])
                    if nwin:
                        TT('dve', sw[:, 0:nwin], pss_[:, 0:nwin], bias_ap, ALU.add, [Bpss, B_tb], [Bsw])
                    ACT(sw[:, nwin:nk], psc_[:, 0:L], AF.Copy, [Bpsc], [Bsw])
                    S.op('dve', lambda e: e.reduce_max(out=stt[:, 0:1], in_=sw[:, 0:nk], axis=AX.X), [Bsw], [Bst])
                    TS('dve', stt[:, 1:2], stt[:, 0:1], -1.0, None, ALU.mult, None, [Bst], [Bst])
                    ACT(pe_[:, 0:nk], sw[:, 0:nk], AF.Exp, [Bsw, Bst], [Bpe, Bst], bias=stt[:, 1:2], scale=1.0, accum=stt[:, 2:3])
                    S.op('dve', lambda e: e.reciprocal(out=stt[:, 3:4], in_=stt[:, 2:3]), [Bst], [Bst])
                    TS('dve', pn[:, 0:nk], pe_[:, 0:nk], stt[:, 3:4], None, ALU.mult, None, [Bpe, Bst], [Bpn])
                    nj = nk // 128
                    for j in range(nj):
                        TR(pst_[:, j, :], pn[:, j * 128:(j + 1) * 128], identb[:, :], [Bpn, B_id], [Bpst])
                    CP('dve', ptb[:, 0:nj, :], pst_[:, 0:nj, :], [Bpst], [Bptb])
                    for hh in range(2):
                        for j in range(nj):
                            MM(pso_[:, hh, :], v_fn(hh, j), ptb[:, j, hh * 64:(hh + 1) * 64], j == 0, j == nj - 1,
                               [Bv, B_vc, Bptb], [Bpso])
                    ACT(o_ap, pso_[:], AF.Copy, [Bpso], [Bo])

                state = {'rs': -1}

                def p5_load(r):
                    res = {}
                    if r % 8 == 0:
                        qt, Bq = qts.next()
                        DMA('sp', qt[:], PT[CH_Q:CH_Q + 8, :, L + r * 64:L + (r + 8) * 64].rearrange("c p t -> p c t"), (), [Bq])
                        state['q'] = (qt, Bq)
                    rs = min(max(r - 4, 0), ROWS - 8)
                    if rs != state['rs']:
                        kw, Bk = kws.next()
                        vw, Bv = vws.next()
                        DMA('sp', kw[:], PT[CH_K:CH_K + 8, :, L + rs * 64:L + rs * 64 + 512].rearrange("c p t -> p c t"), (), [Bk])
                        DMA('sp', vw[:], VT[L + rs * 64:L + rs * 64 + 512, :].rearrange("(s p) c -> p s c", p=128), (), [Bv])
                        state['rs'] = rs
                        state['kv'] = (kw, Bk, vw, Bv)
                    return state['q'] + state['kv'] + (rs,)

                def p5_comp(r, ld):
                    qt, Bq, kw, Bk, vw, Bv, rs = ld
                    rr = r % 8
                    if rr == 0:
                        state['ao'] = aos.next()
                    ao, Bao = state['ao']
                    dr0 = rs - r + 7
                    for pr_ in range(4):
                        h0 = 2 * pr_

                        def v_fn(hh, j, h0=h0, vw=vw):
                            h = h0 + hh
                            if j < 4:
                                return vw[:, j, h * 128:(h + 1) * 128]
                            return vcx[:, j - 4, h * 128:(h + 1) * 128]
                        attn_unit(lambda hh, h0=h0: qt[:, h0 + hh, rr * 64:(rr + 1) * 64], Bq,
                                  lambda hh, h0=h0: kw[:, h0 + hh, :], Bk,
                                  lambda hh, h0=h0: kcT[:, h0 + hh, :], v_fn, Bv, 512,
                                  tb[:, pr_, dr0 * 64:(dr0 + 8) * 64], ao[:, h0:h0 + 2, rr * 64:(rr + 1) * 64], Bao)
                    if rr == 7:
                        r0 = r - 7
                        DMA('sp', AT[:, :, L + r0 * 64:L + (r0 + 8) * 64].rearrange("c p t -> p c t"), ao[:], [Bao], ())
                pipeline(list(range(ROWS)), p5_load, p5_comp, ahead=1)
                if not last:
                    qt, Bq = qts.next()
                    DMA('sp', qt[:, :, 0:L], PT[CH_Q:CH_Q + 8, :, 0:L].rearrange("c p t -> p c t"), (), [Bq])
                    ao, Bao = aos.next()
                    for qq in range(L // 64):
                        for pr_ in range(4):
                            h0 = 2 * pr_

                            def v_fn(hh, j, h0=h0):
                                h = h0 + hh
                                return vcx[:, j, h * 128:(h + 1) * 128]
                            attn_unit(lambda hh, h0=h0: qt[:, h0 + hh, qq * 64:(qq + 1) * 64], Bq, None, None,
                                      lambda hh, h0=h0: kcT[:, h0 + hh, :], v_fn, B_vc, 0, None,
                                      ao[:, h0:h0 + 2, qq * 64:(qq + 1) * 64], Bao)
                    DMA('sp', AT[:, :, 0:L].rearrange("c p t -> p c t"), ao[:, :, 0:L], [Bao], ())
                phase_end()
            if done(l, 'P5'):
                finished = True
                break

            with ExitStack() as st:
                wouts = []
                for nm, wsrc in (("wco", w_conv_out), ("wpo", w_pool_out), ("wao", w_attn_out)):
                    wt = sbt(st, nm, [128, 8, D], BF16)
                    Bw = Buf()
                    for hh in range(2):
                        CDMA(wt[:, :, hh * 1024:(hh + 1) * 1024],
                             wsrc[l].rearrange("(kc p) c -> p kc c", p=128)[:, :, hh * 1024:(hh + 1) * 1024], [Bw])
                    wouts.append((wt, Bw))
                brs = [Ring([sbt(st, f"br{b}_{i}", [128, 8, 512], BF16) for i in range(2)]) for b in range(3)]
                gts = Ring([sbt(st, f"gt{i}", [128, 3, 512], BF16) for i in range(3)])
                mbs = Ring([sbt(st, f"mb{i}", [128, KC, 512], BF16) for i in range(2)])
                tmps = Ring([sbt(st, f"tm{i}", [128, 3, 512], F32) for i in range(2)])
                pss = Ring([pst(st, f"py{i}", [128, 512], F32) for i in range(6)])

                def p6a_load(blk):
                    t0, n = blk
                    bt = []
                    for b, srcT in enumerate((CT, PO, AT)):
                        t_, B_ = brs[b].next()
                        DMA('sp', t_[:, :, 0:n], srcT[:, :, t0:t0 + n].rearrange("c p t -> p c t"), (), [B_])
                        bt.append((t_, B_))
                    return bt

                def p6a_comp(blk, bt):
                    t0, n = blk
                    mb, Bmb = mbs.next()

                    def gload(fc):
                        gt, Bg = gts.next()
                        for b in range(3):
                            DMA('sp', gt[:, b, 0:n], PT[CH_G + b * 16 + fc, :, t0:t0 + n], (), [Bg])
                        return gt, Bg

                    def gcomp(fc, ld):
                        gt, Bg = ld
                        tm, Btm = tmps.next()
                        for b in range(3):
                            ps, Bp = pss.next()
                            wt, Bw = wouts[b]
                            for kc in range(8):
                                MM(ps[:, 0:n], wt[:, kc, fc * 128:(fc + 1) * 128], bt[b][0][:, kc, 0:n], kc == 0, kc == 7,
                                   [Bw, bt[b][1]], [Bp])
                            TT('dve', tm[:, b, 0:n], ps[:, 0:n], gt[:, b, 0:n], ALU.mult, [Bp, Bg], [Btm])
                        TT('pool', tm[:, 0, 0:n], tm[:, 0, 0:n], tm[:, 1, 0:n], ALU.add, [Btm], [Btm])
                        TT('pool', mb[:, fc, 0:n], tm[:, 0, 0:n], tm[:, 2, 0:n], ALU.add, [Btm], [Bmb])
                    pipeline(list(range(KC)), gload, gcomp, ahead=2)
                    DMA('sp', MT[:, :, t0:t0 + n].rearrange("c p t -> p c t"), mb[:, :, 0:n], [Bmb], ())
                pipeline(blocks(last), p6a_load, p6a_comp, ahead=1)
                phase_end()

            with ExitStack() as st:
                wo = sbt(st, "wo", [128, KC, D], BF16)
                B_wo = Buf()
                for hh in range(4):
                    CDMA(wo[:, :, hh * 512:(hh + 1) * 512],
                         w_o[l].rearrange("(kc p) c -> p kc c", p=128)[:, :, hh * 512:(hh + 1) * 512], [B_wo])
                g1 = sbt(st, "g1", [128, 2, D], F32)
                B_g1 = Buf()
                for j in range(2):
                    DMA('sp', g1[:, j, :], MODROW[l, j:j + 1, 2 * D:3 * D].to_broadcast([128, D]), (), [B_g1])
                mbs = Ring([sbt(st, f"mb{i}", [128, KC, 512], BF16) for i in range(2)])
                xts = Ring([sbt(st, f"xt{i}", [128, D], F32) for i in range(3)])
                xos = Ring([sbt(st, f"xo{i}", [128, D], F32) for i in range(2)])
                pss = Ring([pst(st, f"pw{i}", [128, 512], F32) for i in range(4)])
                items = [(t0, n, sub) for (t0, n) in blocks(last) for sub in range(n // 128)]
                state = {}

                def p6b_load(it):
                    t0, n, sub = it
                    if sub == 0:
                        mb, Bmb = mbs.next()
                        DMA('sp', mb[:, :, 0:n], MT[:, :, t0:t0 + n].rearrange("c p t -> p c t"), (), [Bmb])
                        state['mb'] = (mb, Bmb)
                    xt, Bx = xts.next()
                    DMA('sp', xt[:], src_rows(l, t0 + sub * 128, 128), (), [Bx])
                    return state['mb'] + (xt, Bx)

                def p6b_comp(it, ld):
                    t0, n, sub = it
                    mb, Bmb, xt, Bx = ld
                    j = 1 if t0 < L else 0
                    xo, Bxo = xos.next()
                    for cb in range(4):
                        ps, Bp = pss.next()
                        for kc in range(KC):
                            MM(ps[:], mb[:, kc, sub * 128:(sub + 1) * 128], wo[:, kc, cb * 512:(cb + 1) * 512],
                               kc == 0, kc == KC - 1, [Bmb, B_wo], [Bp])
                        TT('dve', xo[:, cb * 512:(cb + 1) * 512], ps[:], g1[:, j, cb * 512:(cb + 1) * 512], ALU.mult,
                           [Bp, B_g1], [Bxo])
                        TT('pool', xo[:, cb * 512:(cb + 1) * 512], xo[:, cb * 512:(cb + 1) * 512],
                           xt[:, cb * 512:(cb + 1) * 512], ALU.add, [Bxo, Bx], [Bxo])
                    DMA('sp', XA[t0 + sub * 128:t0 + (sub + 1) * 128, :], xo[:], [Bxo], ())
                pipeline(items, p6b_load, p6b_comp, ahead=1)
                phase_end()
            if done(l, 'P6'):
                finished = True
                break

            with ExitStack() as st:
                a2 = sbt(st, "a2", [128, 2, D], F32)
                b2 = sbt(st, "b2", [128, 2, D], F32)
                n2 = sbt(st, "n2", [128, D], F32)
                B_a2 = Buf()
                DMA('sp', n2[:], norm2row[l].to_broadcast([128, D]), (), [B_a2])
                for j in range(2):
                    DMA('sp', a2[:, j, :], MODROW[l, j:j + 1, 4 * D:5 * D].to_broadcast([128, D]), (), [B_a2])
                    DMA('sp', b2[:, j, :], MODROW[l, j:j + 1, 3 * D:4 * D].to_broadcast([128, D]), (), [B_a2])
                for j in range(2):
                    TS('dve', a2[:, j, :], a2[:, j, :], 1.0, None, ALU.add, None, [B_a2], [B_a2])
                    TT('dve', a2[:, j, :], a2[:, j, :], n2[:], ALU.mult, [B_a2], [B_a2])
                wr = sbt(st, "wr", [128, KC, NE], BF16)
                B_wr = Buf()
                CDMA(wr[:], w_router[l].rearrange("(kc p) e -> p kc e", p=128), [B_wr])
                xts = Ring([sbt(st, f"xt{i}", [128, D], F32) for i in range(3)])
                junk = sbt(st, "junk", [128, D], BF16)
                B_junk = Buf()
                hfs = Ring([sbt(st, f"hf{i}", [128, D], F32) for i in range(2)])
                hbs = Ring([sbt(st, f"h2b{i}", [128, D], BF16) for i in range(2)])
                hts = Ring([sbt(st, f"h2t{i}", [128, KC, 128], BF16) for i in range(2)])
                sss = Ring([sbt(st, f"ss{i}", [128, 4], F32) for i in range(2)])
                lgs = Ring([sbt(st, f"lg{i}", [128, 2, NE], F32) for i in range(2)])
                afs = Ring([sbt(st, f"af{i}", [NE, 128], F32) for i in range(2)])
                pts = Ring([pst(st, f"pt{i}", [128, KC, 128], BF16) for i in range(2)])
                pls = Ring([pst(st, f"pl{i}", [128, NE], F32) for i in range(2)])
                pas = Ring([pst(st, f"pa{i}", [NE, 128], F32) for i in range(2)])

                def p7_load(t):
                    xt, Bx = xts.next()
                    DMA('sp', xt[:], XA[t * 128:(t + 1) * 128, :], (), [Bx])
                    return xt, Bx

                def p7_comp(t, ld):
                    xt, Bx = ld
                    j = 1 if t < 2 else 0
                    hf, Bhf = hfs.next()
                    hb, Bhb = hbs.next()
                    ht, Bht = hts.next()
                    ss, Bs = sss.next()
                    lg, Blg = lgs.next()
                    af, Baf = afs.next()
                    pt, Bp = pts.next()
                    pl, Bpl = pls.next()
                    pa, Bpa = pas.next()
                    ACT(junk[:], xt[:], AF.Square, [Bx], [B_junk, Bs], accum=ss[:, 0:1])
                    ACT(ss[:, 1:2], ss[:, 0:1], AF.Sqrt, [Bs], [Bs], bias=EPS, scale=1.0 / D)
                    S.op('dve', lambda e: e.reciprocal(out=ss[:, 1:2], in_=ss[:, 1:2]), [Bs], [Bs])
                    STT('dve', hf[:], xt[:], ss[:, 1:2], a2[:, j, :], ALU.mult, ALU.mult, [Bx, Bs, B_a2], [Bhf])
                    TT('pool', hb[:], hf[:], b2[:, j, :], ALU.add, [Bhf, B_a2], [Bhb])
                    DMA('sp', H2[t * 128:(t + 1) * 128, :], hb[:], [Bhb], ())
                    for fc in range(KC):
                        TR(pt[:, fc, :], hb[:, fc * 128:(fc + 1) * 128], identb[:], [Bhb, B_id], [Bp])
                    CP('dve', ht[:, 0:8, :], pt[:, 0:8, :], [Bp], [Bht])
                    ACT(ht[:, 8:16, :], pt[:, 8:16, :], AF.Copy, [Bp], [Bht])
                    for kc in range(KC):
                        MM(pl[:], ht[:, kc, :], wr[:, kc, :], kc == 0, kc == KC - 1, [Bht, B_wr], [Bpl])
                    S.op('dve', lambda e: e.reduce_max(out=ss[:, 2:3], in_=pl[:], axis=AX.X), [Bpl], [Bs])
                    TS('dve', ss[:, 2:3], ss[:, 2:3], -1.0, None, ALU.mult, None, [Bs], [Bs])
                    ACT(lg[:, 0, :], pl[:], AF.Exp, [Bpl, Bs], [Blg, Bs], bias=ss[:, 2:3], scale=1.0, accum=ss[:, 3:4])
                    S.op('dve', lambda e: e.reciprocal(out=ss[:, 3:4], in_=ss[:, 3:4]), [Bs], [Bs])
                    TS('dve', lg[:, 1, :], lg[:, 0, :], ss[:, 3:4], None, ALU.mult, None, [Blg, Bs], [Blg])
                    TR(pa[:], lg[:, 1, :], identf[:], [Blg, B_id], [Bpa])
                    CP('dve', af[:], pa[:], [Bpa], [Baf])
                    DMA('sp', AFFT[:, t * 128:(t + 1) * 128], af[:], [Baf], ())
                pipeline(tiles_moe, p7_load, p7_comp, ahead=2)
                phase_end()
            if done(l, 'P7'):
                finished = True
                break

            with ExitStack() as st:
                affT = sbt(st, "affT", [NE, NT], F32)
                junkm = sbt(st, "junkm", [NE, NT], F32)
                msk = sbt(st, "msk", [NE, NT], F32)
                pos = sbt(st, "pos", [NE, NT], F32)
                B_aff, B_jm, B_msk, B_pos = Buf(), Buf(), Buf(), Buf()
                s_lo = L if last else 0
                DMA('sp', affT[:, s_lo:NT], AFFT[:, s_lo:NT], (), [B_aff])
                sc = sbt(st, "sc", [NE, 8], F32)
                B_sc = Buf()
                seg_list = [(L, T, CAPX, 0.0)] if last else [(0, L, CAPC, float(CAPX)), (L, T, CAPX, 0.0)]
                for (s0, n, kk, base) in seg_list:
                    lo, hi, mid, cnt, cc, dd = [sc[:, i:i + 1] for i in range(6)]
                    MS('dve', lo, 0.0, [B_sc])
                    MS('dve', hi, 1.0, [B_sc])
                    for it in range(32):
                        TT('dve', mid, lo, hi, ALU.add, [B_sc], [B_sc])
                        TS('dve', mid, mid, 0.5, None, ALU.mult, None, [B_sc], [B_sc])
                        TS('dve', junkm[:, s0:s0 + n], affT[:, s0:s0 + n], mid, None, ALU.is_ge, ALU.add,
                           [B_aff, B_sc], [B_jm, B_sc], accum=cnt)
                        TS('dve', cc, cnt, float(kk) - 0.5, None, ALU.is_ge, None, [B_sc], [B_sc])
                        TT('dve', dd, mid, lo, ALU.subtract, [B_sc], [B_sc])
                        STT('dve', lo, dd, cc, lo, ALU.mult, ALU.add, [B_sc], [B_sc])
                        TT('dve', dd, hi, mid, ALU.subtract, [B_sc], [B_sc])
                        STT('dve', hi, dd, cc, mid, ALU.mult, ALU.add, [B_sc], [B_sc])
                    TS('dve', msk[:, s0:s0 + n], affT[:, s0:s0 + n], lo, None, ALU.is_ge, None, [B_aff, B_sc], [B_msk])
                    S.op('dve', (lambda o, i: (lambda e: e.tensor_tensor_scan(out=o, data0=i, data1=i, initial=0.0,
                                                                              op0=ALU.add, op1=ALU.max)))(
                        pos[:, s0:s0 + n], msk[:, s0:s0 + n]), [B_msk], [B_pos])
                    TS('dve', junkm[:, s0:s0 + n], pos[:, s0:s0 + n], float(kk) + 0.5, None, ALU.is_le, None, [B_pos], [B_jm])
                    TT('dve', msk[:, s0:s0 + n], msk[:, s0:s0 + n], junkm[:, s0:s0 + n], ALU.mult, [B_msk, B_jm], [B_msk])
                    TS('dve', pos[:, s0:s0 + n], pos[:, s0:s0 + n], base - 1.0 - BIG, None, ALU.add, None, [B_pos], [B_pos])
                    TT('dve', pos[:, s0:s0 + n], pos[:, s0:s0 + n], msk[:, s0:s0 + n], ALU.mult, [B_pos, B_msk], [B_pos])
                    TS('dve', pos[:, s0:s0 + n], pos[:, s0:s0 + n], BIG, None, ALU.add, None, [B_pos], [B_pos])
                    TT('dve', msk[:, s0:s0 + n], msk[:, s0:s0 + n], affT[:, s0:s0 + n], ALU.mult, [B_msk, B_aff], [B_msk])
                ptk = Ring([pst(st, f"ptk{i}", [128, 2, NE], F32) for i in range(2)])
                idxf = sbt(st, "idxf", [128, NTILE, NE], F32)
                B_if = Buf()
                for t in tiles_moe:
                    p_, Bp_ = ptk.next()
                    TR(p_[:, 0, :], pos[:, t * 128:(t + 1) * 128], identf[0:NE, 0:NE], [B_pos, B_id], [Bp_])
                    TR(p_[:, 1, :], msk[:, t * 128:(t + 1) * 128], identf[0:NE, 0:NE], [B_msk, B_id], [Bp_])
                    CP('dve', idxf[:, t, :], p_[:, 0, :], [Bp_], [B_if])
                    CP('dve', gmt[:, t, :], p_[:, 1, :], [Bp_], [B_rt])
                CP('dve', idxi[:], idxf[:], [B_if], [B_rt])
                if 'IDXD' in dbg:
                    DMA('sp', IDXD.rearrange("(t p) e -> p t e", p=128), idxf[:], [B_if], ())
                    DMA('sp', AFFD.rearrange("(t p) e -> p t e", p=128), gmt[:], [B_rt], ())
                phase_end()
            if done(l, 'P8'):
                finished = True
                break

            with ExitStack() as st:
                hbs = Ring([sbt(st, f"h2b{i}", [128, D], BF16) for i in range(3)])

                def p9_load(t):
                    hb, Bhb = hbs.next()
                    DMA('sp', hb[:], H2[t * 128:(t + 1) * 128, :], (), [Bhb])
                    return hb, Bhb

                def p9_comp(t, ld):
                    hb, Bhb = ld
                    for e_ in range(NE):
                        S.dma('pool', (lambda o, ia, i: (lambda e: e.indirect_dma_start(
                            out=o, out_offset=bass.IndirectOffsetOnAxis(ap=ia, axis=0), in_=i, in_offset=None,
                            bounds_check=S.getreg(e, nslot - 1), oob_is_err=False)))(XS[e_], idxi[:, t, e_:e_ + 1], hb[:, :]),
                            [Bhb, B_rt], ())
                pipeline(tiles_moe, p9_load, p9_comp, ahead=2)
                phase_end()
            if done(l, 'P9'):
                finished = True
                break

            with ExitStack() as st:
                nst = (nslot + 127) // 128
                xrs = Ring([sbt(st, f"xr{i}", [128, D], BF16) for i in range(3)])
                xsT = Ring([sbt(st, f"xsT{i}", [128, KC, NSLOT], BF16) for i in range(2)])
                hT = Ring([sbt(st, f"hT{i}", [128, KC, NSLOT], BF16) for i in range(1)])
                wgs = Ring([sbt(st, f"wg{i}", [128, KC, 512], BF16) for i in range(2)])
                wus = Ring([sbt(st, f"wu{i}", [128, KC, 512], BF16) for i in range(2)])
                wds = Ring([sbt(st, f"wd{i}", [128, KC, 512], BF16) for i in range(2)])
                sgs = Ring([sbt(st, f"sg{i}", [128, NSLOT], F32) for i in range(2)])
                yos = Ring([sbt(st, f"yo{i}", [128, 512], F32) for i in range(3)])
                ptr = Ring([pst(st, f"ptr{i}", [128, 4, 128], BF16) for i in range(2)])
                pg = Ring([pst(st, f"pg{i}", [128, 2, 512], F32) for i in range(1)])
                pu = Ring([pst(st, f"pu{i}", [128, 2, 512], F32) for i in range(1)])
                pd = Ring([pst(st, f"pd{i}", [128, 512], F32) for i in range(2)])
                nsp = [(0, min(512, nslot))] + ([(512, nslot - 512)] if nslot > 512 else [])
                jobs = []
                for e_ in range(NE):
                    for fb in range(4):
                        jobs.append((e_, 'gu', fb))
                    for db in range(4):
                        jobs.append((e_, 'd', db))
                state = {}

                def p10_load(job):
                    e_, kind, blk = job
                    if kind == 'gu':
                        wg, Bwg = wgs.next()
                        wu, Bwu = wus.next()
                        CDMA(wg[:], w_e_gate[l, e_].rearrange("(kc p) f -> p kc f", p=128)[:, :, blk * 512:(blk + 1) * 512], [Bwg])
                        CDMA(wu[:], w_e_up[l, e_].rearrange("(kc p) f -> p kc f", p=128)[:, :, blk * 512:(blk + 1) * 512], [Bwu])
                        return (wg, Bwg, wu, Bwu)
                    wd, Bwd = wds.next()
                    CDMA(wd[:], w_e_down[l, e_].rearrange("(kc p) d -> p kc d", p=128)[:, :, blk * 512:(blk + 1) * 512], [Bwd])
                    return (wd, Bwd)

                def p10_comp(job, ld):
                    e_, kind, blk = job
                    if kind == 'gu' and blk == 0:
                        xT, BxT = xsT.next()
                        for s_ in range(nst):
                            n = min(128, nslot - s_ * 128)
                            xr, Bxr = xrs.next()
                            DMA('sp', xr[0:n, :], XS[e_][s_ * 128:s_ * 128 + n, :], (), [Bxr])
                            for q4 in range(4):
                                p_, Bp_ = ptr.next()
                                for jj in range(4):
                                    fc = q4 * 4 + jj
                                    TR(p_[:, jj, 0:n], xr[0:n, fc * 128:(fc + 1) * 128], identb[0:n, 0:n], [Bxr, B_id], [Bp_])
                                if q4 % 2 == 0:
                                    CP('dve', xT[:, q4 * 4:(q4 + 1) * 4, s_ * 128:s_ * 128 + n], p_[:, :, 0:n], [Bp_], [BxT])
                                else:
                                    ACT(xT[:, q4 * 4:(q4 + 1) * 4, s_ * 128:s_ * 128 + n], p_[:, :, 0:n], AF.Copy, [Bp_], [BxT])
                        state['xT'] = (xT, BxT)
                        state['h'] = hT.next()
                    xT, BxT = state['xT']
                    h_, Bh_ = state['h']
                    if kind == 'gu':
                        wg, Bwg, wu, Bwu = ld
                        for jj in range(4):
                            fch = blk * 4 + jj
                            g_, Bg_ = pg.next()
                            u_, Bu_ = pu.next()
                            sg, Bsg = sgs.next()
                            for (c0, cn) in nsp:
                                bi = 0 if c0 == 0 else 1
                                for kc in range(KC):
                                    MM(g_[:, bi, 0:cn], wg[:, kc, jj * 128:(jj + 1) * 128], xT[:, kc, c0:c0 + cn],
                                       kc == 0, kc == KC - 1, [Bwg, BxT], [Bg_])
                                for kc in range(KC):
                                    MM(u_[:, bi, 0:cn], wu[:, kc, jj * 128:(jj + 1) * 128], xT[:, kc, c0:c0 + cn],
                                       kc == 0, kc == KC - 1, [Bwu, BxT], [Bu_])
                                ACT(sg[:, c0:c0 + cn], g_[:, bi, 0:cn], AF.Silu, [Bg_], [Bsg])
                                TT('dve', h_[:, fch, c0:c0 + cn], sg[:, c0:c0 + cn], u_[:, bi, 0:cn], ALU.mult, [Bsg, Bu_], [Bh_])
                    else:
                        wd, Bwd = ld
                        for s_ in range(nst):
                            n = min(128, nslot - s_ * 128)
                            p_, Bp_ = pd.next()
                            yo, Byo = yos.next()
                            for fc in range(KC):
                                MM(p_[0:n, :], h_[:, fc, s_ * 128:s_ * 128 + n], wd[:, fc, :], fc == 0, fc == KC - 1,
                                   [Bh_, Bwd], [Bp_])
                            if s_ % 2 == 0:
                                CP('dve', yo[0:n, :], p_[0:n, :], [Bp_], [Byo])
                            else:
                                ACT(yo[0:n, :], p_[0:n, :], AF.Copy, [Bp_], [Byo])
                            DMA('sp', YS[e_][s_ * 128:s_ * 128 + n, blk * 512:(blk + 1) * 512], yo[0:n, :], [Byo], ())
                pipeline(jobs, p10_load, p10_comp, ahead=1)
                phase_end()
            if done(l, 'P10'):
                finished = True
                break

            with ExitStack() as st:
                g2 = sbt(st, "g2", [128, 2, D], F32)
                B_g2 = Buf()
                for j in range(2):
                    DMA('sp', g2[:, j, :], MODROW[l, j:j + 1, 5 * D:6 * D].to_broadcast([128, D]), (), [B_g2])
                fn = sbt(st, "fn", [128, D], F32)
                DMA('sp', fn[:], fnorm_row.to_broadcast([128, D]), (), [B_g2])
                gbs = Ring([sbt(st, f"gb{i}", [128, D], F32) for i in range(4)])
                for (gb, Bgb) in gbs.t:
                    MS('dve', gb[:], 0.0, [Bgb])
                accs = Ring([sbt(st, f"acc{i}", [128, D], F32) for i in range(2)])
                xts = Ring([sbt(st, f"xt{i}", [128, D], F32) for i in range(3)])
                junk = sbt(st, "junk", [128, D], BF16)
                B_junk = Buf()
                sss = Ring([sbt(st, f"ss{i}", [128, 2], F32) for i in range(2)])

                def p11_load(t):
                    xt, Bx = xts.next()
                    DMA('sp', xt[:], XA[t * 128:(t + 1) * 128, :], (), [Bx])
                    return xt, Bx

                def p11_comp(t, ld):
                    xt, Bx = ld
                    j = 1 if t < 2 else 0
                    acc, Bacc = accs.next()
                    for e_ in range(NE):
                        gb, Bgb = gbs.next()
                        S.dma('pool', (lambda o, ia, i: (lambda e: e.indirect_dma_start(
                            out=o, out_offset=None, in_=i, in_offset=bass.IndirectOffsetOnAxis(ap=ia, axis=0),
                            bounds_check=S.getreg(e, nslot - 1), oob_is_err=False)))(gb[:, :], idxi[:, t, e_:e_ + 1], YS[e_]),
                            [B_rt], [Bgb])
                        if e_ == 0:
                            TS('dve', acc[:], gb[:], gmt[:, t, e_:e_ + 1], None, ALU.mult, None, [Bgb, B_rt], [Bacc])
                        else:
                            STT('dve', acc[:], gb[:], gmt[:, t, e_:e_ + 1], acc[:], ALU.mult, ALU.add, [Bgb, B_rt, Bacc], [Bacc])
                    TT('dve', acc[:], acc[:], g2[:, j, :], ALU.mult, [Bacc, B_g2], [Bacc])
                    TT('dve', acc[:], acc[:], xt[:], ALU.add, [Bacc, Bx], [Bacc])
                    if not last:
                        DMA('sp', XB[t * 128:(t + 1) * 128, :], acc[:], [Bacc], ())
                    else:
                        ss, Bs = sss.next()
                        ACT(junk[:], acc[:], AF.Square, [Bacc], [B_junk, Bs], accum=ss[:, 0:1])
                        ACT(ss[:, 1:2], ss[:, 0:1], AF.Sqrt, [Bs], [Bs], bias=EPS, scale=1.0 / D)
                        S.op('dve', lambda e: e.reciprocal(out=ss[:, 1:2], in_=ss[:, 1:2]), [Bs], [Bs])
                        STT('dve', xt[:], acc[:], ss[:, 1:2], fn[:], ALU.mult, ALU.mult, [Bacc, Bs, B_g2, Bx], [Bx])
                        DMA('sp', out[(t - 2) * 128:(t - 1) * 128, :], xt[:], [Bx], ())
                pipeline(tiles_moe, p11_load, p11_comp, ahead=2)
                phase_end()
            if done(l, 'P11'):
                finished = True
                break
        S.barrier()
        S.flush()
        print("build: instructions", S.ninst, "max dma sem", max(S.dcnt), "sems", len(S.handles))
        assert max(S.dcnt) < 2040 and len(S.handles) <= 100
    nc._declared_inputs = declared
    return nc


def prep_inputs(inp, s):
    f = np.float32
    c2 = np.stack([inp['c'][s], inp['c_ctx']], axis=-1)
    cT = np.ascontiguousarray(c2.reshape(KC, 128, 2).transpose(1, 0, 2).reshape(128, 2 * KC)).astype(f)
    m = {
        "x": np.ascontiguousarray(inp['x'][s]),
        "ctx": np.ascontiguousarray(inp['ctx'][s]),
        "cT": cT,
    }
    return m


def shared_inputs(inp):
    f = np.float32
    import ml_dtypes
    sh = {}
    sh["w_mod"] = inp['w_mod']
    sh["b_modT"] = np.ascontiguousarray(inp['b_mod'].reshape(DEPTH, 96, 128).transpose(0, 2, 1))
    sh["norm1T"] = np.ascontiguousarray(inp['norm1'].reshape(DEPTH, KC, 128).transpose(0, 2, 1))
    sh["norm2"] = np.ascontiguousarray(inp['norm2'].reshape(DEPTH, 1, D))
    sh["w_in"] = inp['w_in']
    sh["conv_wT"] = np.ascontiguousarray(inp['conv_w'].reshape(DEPTH, 3, 8, 128).transpose(0, 3, 2, 1).reshape(DEPTH, 128, 24))
    sh["pool_w"] = inp['pool_w']
    sh["pool_scaleT"] = np.ascontiguousarray(inp['pool_scale'].reshape(DEPTH, 8, 128).transpose(0, 2, 1))
    rc = np.zeros((4, NT), f)
    for g, w in enumerate((2, 4, 8, 16)):
        for (s0, n) in ((0, L), (L, T)):
            t = np.arange(n)
            lo = np.clip(t - w // 2, 0, n)
            hi = np.clip(t - w // 2 + w, 0, n)
            rc[g, s0:s0 + n] = (1.0 / (hi - lo)).astype(f)
    sh["rc_tab"] = rc
    col = np.arange(GW)
    cs = np.clip(col - 8, 0, GW - 16)
    kcol = np.arange(GW)
    inwin = (kcol[None, :] >= cs[:, None]) & (kcol[None, :] < cs[:, None] + 16)
    dc = np.clip(kcol[None, :] - col[:, None] + 15, 0, 30)
    rpb = inp['rpb']
    tabs = rpb[:, :, :, dc]
    tabs = np.where(inwin[None, None, None], tabs, f(NEG))
    tabs = tabs.reshape(DEPTH, 4, 2, 15, GW, GW)
    tabs = np.ascontiguousarray(tabs.transpose(0, 2, 4, 1, 3, 5)).reshape(DEPTH, 2 * GW, 4 * 15 * GW)
    sh["bias_tab"] = tabs.astype(ml_dtypes.bfloat16)
    for k in ("w_conv_out", "w_pool_out", "w_attn_out", "w_o", "w_router", "w_e_gate", "w_e_up", "w_e_down"):
        sh[k] = inp[k]
    sh["final_norm"] = np.ascontiguousarray(inp['final_norm'].reshape(1, D))
    return sh


def kernel(**inputs):
    inp = {k: np.asarray(v) for k, v in inputs.items()}
    nc = build({})
    sh = shared_inputs(inp)
    in_maps = []
    for core in range(NCORES):
        m = dict(sh)
        m.update(prep_inputs(inp, core % 4))
        in_maps.append(m)
    res = run_bass_kernel_spmd(nc, in_maps, core_ids=list(range(NCORES)))
    outs = [np.asarray(res.results[i]["out"]).reshape(T, D) for i in range(4)]
    return np.stack(outs, axis=0).astype(np.float32)
```

```python
import numpy as np
from contextlib import ExitStack
import concourse.bass as bass
import concourse.mybir as mybir
from concourse.bass_utils import run_bass_kernel_spmd

F32 = mybir.dt.float32
BF16 = mybir.dt.bfloat16
I32 = mybir.dt.int32
AF = mybir.ActivationFunctionType
ALU = mybir.AluOpType
AX = mybir.AxisListType

D = 2048
KC = 16
T = 4096
L = 256
NT = T + L
NTILE = NT // 128
GW = 64
ROWS = T // GW
NE = 16
CAPX = 512
CAPC = 32
NSLOT = CAPX + CAPC
IN_COLS = 13312
NCH = IN_COLS // 128
CH_AB, CH_AC, CH_AX, CH_POOL, CH_Q, CH_K, CH_V, CH_G = 0, 8, 16, 24, 32, 40, 48, 56
EPS = 1e-6
BIG = 100000.0
NEG = -30000.0
DEPTH = 2
NCORES = 4


class Buf:
    __slots__ = ('w', 'r')

    def __init__(self):
        self.w = {}
        self.r = {}


class Sched:
    ENG = ('pe', 'act', 'dve', 'pool', 'sp')
    ROT = 30000

    def __init__(self, nc, stack, n_dma=64):
        self.nc = nc
        self.stack = stack
        self.ops = {e: [] for e in self.ENG}
        self.handles = []
        self.cur = {}
        self.cnt = {}
        self.mine = {e: set() for e in self.ENG}
        self.waited = {e: {} for e in self.ENG}
        self.final = {}
        self.spare = {e: [self._alloc(f"e_{e}_{i}") for i in range(6 if e == 'pe' else 2)] for e in self.ENG}
        for e in self.ENG:
            self._new_eng_sem(e)
        self.dkeys = []
        self.dcnt = []
        for i in range(n_dma):
            self.dkeys.append(self._alloc(f"dq{i}"))
            self.dcnt.append(0)
        self.drr = 0
        self.ninst = 0

    def _alloc(self, name):
        h = self.stack.enter_context(self.nc.semaphore(name))
        self.handles.append(h)
        return len(self.handles) - 1

    def _new_eng_sem(self, e):
        if e in self.cur:
            self.final[self.cur[e]] = self.cnt[e]
        k = self.spare[e].pop(0)
        self.cur[e] = k
        self.cnt[e] = 0
        self.mine[e].add(k)

    def _wait(self, e, k, v):
        w = self.waited[e]
        if w.get(k, 0) >= v:
            return
        w[k] = v
        self.ops[e].append(('w', k, v))

    def _deps(self, e, reads, writes):
        deps = {}
        for b in reads:
            for k, v in b.w.items():
                if deps.get(k, 0) < v:
                    deps[k] = v
        for b in writes:
            for d in (b.w, b.r):
                for k, v in d.items():
                    if deps.get(k, 0) < v:
                        deps[k] = v
        for k, v in deps.items():
            if e == 'pe' and k in self.mine[e]:
                continue
            self._wait(e, k, v)

    def _mark(self, k, v, reads, writes):
        for b in reads:
            if b.r.get(k, 0) < v:
                b.r[k] = v
        for b in writes:
            b.w = {k: v}
            b.r = {}

    def op(self, e, fn, reads=(), writes=()):
        self._deps(e, reads, writes)
        if self.cnt[e] >= self.ROT:
            self._new_eng_sem(e)
        self.cnt[e] += 1
        k = self.cur[e]
        self.ops[e].append(('i', fn, k))
        self._mark(k, self.cnt[e], reads, writes)
        self.ninst += 1

    def dma(self, q, fn, reads=(), writes=()):
        self._deps(q, reads, writes)
        i = self.drr
        self.drr = (self.drr + 1) % len(self.dkeys)
        k = self.dkeys[i]
        prev = self.dcnt[i]
        if prev > 0:
            self._wait(q, k, prev)
        self.dcnt[i] = prev + 16
        self.ops[q].append(('d', fn, k))
        self._mark(k, prev + 16, reads, writes)
        self.ninst += 1

    def barrier(self):
        for e in self.ENG:
            for i, k in enumerate(self.dkeys):
                if self.dcnt[i] > 0:
                    self._wait(e, k, self.dcnt[i])
            for e2 in self.ENG:
                if e2 != e and self.cnt[e2] > 0:
                    self._wait(e, self.cur[e2], self.cnt[e2])
                for k, v in self.final.items():
                    if k in self.mine[e2] and e2 != e:
                        self._wait(e, k, v)

    def getreg(self, eng, val):
        if val not in self.regs:
            self.regs[val] = eng.to_reg(val)
        return self.regs[val]

    def flush(self):
        self.regs = {}
        ops = self.ops
        self.ops = {e: [] for e in self.ENG}
        H = self.handles

        def replay(lst):
            def run(eng):
                for o in lst:
                    if o[0] == 'w':
                        eng.wait_ge(H[o[1]], o[2])
                    elif o[0] == 'i':
                        o[1](eng).then_inc(H[o[2]], 1)
                    else:
                        o[1](eng).then_inc(H[o[2]], 16)
            return run
        self.nflush = getattr(self, 'nflush', 0) + 1
        with self.nc.named_scope(f"ph{self.nflush:02d}"):
            with self.nc.Block() as block:
                block.sync(replay(ops['sp']))
                block.tensor(replay(ops['pe']))
                block.scalar(replay(ops['act']))
                block.vector(replay(ops['dve']))
                block.gpsimd(replay(ops['pool']))


class Ring:
    def __init__(self, tiles):
        self.t = [(t, Buf()) for t in tiles]
        self.i = 0

    def next(self):
        r = self.t[self.i]
        self.i = (self.i + 1) % len(self.t)
        return r


def build(cfg):
    nc = bass.Bass("TRN2", target_bir_lowering=False)
    layers = cfg.get('layers', list(range(DEPTH)))
    stop_after = cfg.get('stop_after', None)
    dbg = cfg.get('dbg', [])

    order = ['P0', 'P1', 'P2', 'P4', 'P5', 'P6', 'P7', 'P8', 'P9', 'P10', 'P11']
    needs = {'P0': ['cT', 'w_mod', 'b_modT', 'norm1T'], 'P1': ['x', 'ctx'], 'P2': ['w_in'],
             'P4': ['conv_wT', 'pool_w', 'pool_scaleT', 'rc_tab'], 'P5': ['bias_tab'],
             'P6': ['w_conv_out', 'w_pool_out', 'w_attn_out', 'w_o'], 'P7': ['norm2', 'w_router'],
             'P8': [], 'P9': [], 'P10': ['w_e_gate', 'w_e_up', 'w_e_down'], 'P11': ['final_norm']}
    if stop_after is not None and stop_after[0] == 0 and layers == [0]:
        needed = set()
        for ph in order[:order.index(stop_after[1]) + 1]:
            needed.update(needs[ph])
    else:
        needed = set(sum(needs.values(), []))
    declared = []

    def din(name, shape, dt=F32):
        if name not in needed:
            return None
        declared.append(name)
        return nc.dram_tensor(name, shape, dt, kind="ExternalInput").ap()

    x_in = din("x", [T, D])
    ctx_in = din("ctx", [L, D])
    cT_in = din("cT", [128, 2 * KC])
    w_mod = din("w_mod", [DEPTH, D, 6 * D])
    b_modT = din("b_modT", [DEPTH, 128, 96])
    norm1T = din("norm1T", [DEPTH, 128, KC])
    norm2row = din("norm2", [DEPTH, 1, D])
    w_in = din("w_in", [DEPTH, D, IN_COLS])
    conv_wT = din("conv_wT", [DEPTH, 128, 8 * 3])
    pool_w = din("pool_w", [DEPTH, 4, 256, 256])
    pool_scaleT = din("pool_scaleT", [DEPTH, 128, 8])
    rc_tab = din("rc_tab", [4, NT])
    bias_tab = din("bias_tab", [DEPTH, 128, 4 * 15 * 64], BF16)
    w_conv_out = din("w_conv_out", [DEPTH, 1024, D])
    w_pool_out = din("w_pool_out", [DEPTH, 1024, D])
    w_attn_out = din("w_attn_out", [DEPTH, 1024, D])
    w_o = din("w_o", [DEPTH, D, D])
    w_router = din("w_router", [DEPTH, D, NE])
    w_e_gate = din("w_e_gate", [DEPTH, NE, D, D])
    w_e_up = din("w_e_up", [DEPTH, NE, D, D])
    w_e_down = din("w_e_down", [DEPTH, NE, D, D])
    fnorm_row = din("final_norm", [1, D])
    out = nc.dram_tensor("out", [T, D], F32, kind="ExternalOutput").ap()

    def scr(name, shape, dt):
        kind = "ExternalOutput" if name in dbg else "Internal"
        return nc.dram_tensor(name, shape, dt, kind=kind).ap()

    HT = scr("HT", [KC, 128, NT], BF16)
    PT = scr("PT", [NCH, 128, NT], BF16)
    VT = scr("VT", [NT, 1024], BF16)
    CT = scr("CT", [8, 128, NT], BF16)
    PO = scr("PO", [8, 128, NT], BF16)
    AT = scr("AT", [8, 128, NT], BF16)
    MT = scr("MT", [KC, 128, NT], BF16)
    XA = scr("XA", [NT, D], F32)
    XB = scr("XB", [NT, D], F32)
    H2 = scr("H2", [NT, D], BF16)
    XS = [scr(f"XS{e}", [NSLOT, D], BF16) for e in range(NE)]
    YS = [scr(f"YS{e}", [NSLOT, D], F32) for e in range(NE)]
    MODROW = scr("MODROW", [DEPTH, 2, 96 * 128], F32)
    AFFD = scr("AFFD", [NT, NE], F32)
    AFFT = scr("AFFT", [NE, NT], F32)
    IDXD = scr("IDXD", [NT, NE], F32)

    with ExitStack() as top:
        S = Sched(nc, top)

        uid = [0]

        def sbt(st, name, shape, dt):
            uid[0] += 1
            return st.enter_context(nc.sbuf_tensor(f"s{uid[0]}_{name}", shape, dt))

        def pst(st, name, shape, dt):
            uid[0] += 1
            return st.enter_context(nc.psum_tensor(f"p{uid[0]}_{name}", shape, dt))

        def MM(o, lhsT, rhs, start, stop, r, w):
            S.op('pe', lambda e: e.matmul(o, lhsT=lhsT, rhs=rhs, start=start, stop=stop), r, w)

        def TR(o, i, ident, r, w):
            S.op('pe', lambda e: e.transpose(o, i, ident), r, w)

        def ACT(o, i, func, r, w, bias=None, scale=None, accum=None):
            kw = {}
            if bias is not None:
                kw['bias'] = bias
            if scale is not None:
                kw['scale'] = scale
            if accum is not None:
                kw['accum_out'] = accum
            S.op('act', lambda e: e.activation(out=o, in_=i, func=func, **kw), r, w)

        def TT(eng, o, a, b, op, r, w):
            S.op(eng, lambda e: e.tensor_tensor(out=o, in0=a, in1=b, op=op), r, w)

        def TS(eng, o, a, s1, s2, op0, op1, r, w, accum=None):
            if op1 is None:
                S.op(eng, lambda e: e.tensor_scalar(out=o, in0=a, scalar1=s1, scalar2=None, op0=op0), r, w)
            elif accum is None:
                S.op(eng, lambda e: e.tensor_scalar(out=o, in0=a, scalar1=s1, scalar2=s2, op0=op0, op1=op1), r, w)
            else:
                S.op(eng, lambda e: e.tensor_scalar(out=o, in0=a, scalar1=s1, scalar2=s2, op0=op0, op1=op1, accum_out=accum), r, w)

        def STT(eng, o, a, s, b, op0, op1, r, w):
            S.op(eng, lambda e: e.scalar_tensor_tensor(out=o, in0=a, scalar=s, in1=b, op0=op0, op1=op1), r, w)

        def CP(eng, o, i, r, w):
            S.op(eng, lambda e: e.tensor_copy(out=o, in_=i), r, w)

        def MS(eng, o, val, w):
            S.op(eng, lambda e: e.memset(o, val), (), w)

        def DMA(q, o, i, r=(), w=()):
            S.dma(q, lambda e: e.dma_start(out=o, in_=i), r, w)

        def phase_end():
            S.barrier()
            S.flush()

        def pipeline(items, load, comp, ahead=1):
            loaded = []
            n = len(items)
            for i in range(min(ahead, n)):
                loaded.append(load(items[i]))
            for i in range(n):
                if i + ahead < n:
                    loaded.append(load(items[i + ahead]))
                comp(items[i], loaded[i])
                loaded[i] = None

        def CDMA(o, i, w):
            S.dma('pool', lambda e: e.dma_start(out=o, in_=i), (), w)

        identb = sbt(top, "identb", [128, 128], BF16)
        identf = sbt(top, "identf", [128, 128], F32)
        modT = sbt(top, "modT", [128, 96, 2], F32)
        A1 = sbt(top, "A1", [128, KC, 2], F32)
        idxi = sbt(top, "idxi", [128, NTILE, NE], I32)
        gmt = sbt(top, "gmt", [128, NTILE, NE], F32)
        B_id, B_mod, B_rt = Buf(), Buf(), Buf()
        MS('pool', identf[:], 0.0, [B_id])
        S.op('pool', lambda e: e.affine_select(out=identf[:], in_=identf[:], pattern=[[-1, 128]],
                                               compare_op=ALU.not_equal, fill=1.0, base=0, channel_multiplier=1),
             [B_id], [B_id])
        CP('dve', identb[:], identf[:], [B_id], [B_id])
        phase_end()

        def src_rows(l, t0, n):
            if l == 0:
                if t0 < L:
                    return ctx_in[t0:t0 + n, :]
                return x_in[t0 - L:t0 - L + n, :]
            return XB[t0:t0 + n, :]

        def done(l, ph):
            return stop_after is not None and (l, ph) == tuple(stop_after)

        def blocks(l_last):
            bl = [(L + i * 512, 512) for i in range(8)]
            return bl if l_last else [(0, L)] + bl

        finished = False
        for l in layers:
            if finished:
                break
            last = (l == DEPTH - 1)
            tiles_all = list(range(NTILE))
            tiles_x = list(range(2, NTILE))
            tiles_moe = tiles_x if last else tiles_all
            nslot = CAPX if last else NSLOT

            with ExitStack() as st:
                cT = sbt(st, "cT", [128, 2 * KC], F32)
                scT = sbt(st, "scT", [128, 2 * KC], F32)
                bmT = sbt(st, "bmT", [128, 96], F32)
                wts = Ring([sbt(st, f"wm{i}", [128, KC, 512], F32) for i in range(2)])
                pss = Ring([pst(st, f"pm{i}", [128, 2], F32) for i in range(4)])
                B_c, B_bm = Buf(), Buf()
                DMA('sp', cT[:], cT_in, (), [B_c])
                DMA('sp', bmT[:], b_modT[l], (), [B_bm])
                ACT(scT[:], cT[:], AF.Silu, [B_c], [B_c])
                wv = w_mod[l].rearrange("(kc p) c -> p kc c", p=128)
                def p0_load(cb):
                    wt, Bw = wts.next()
                    DMA('sp', wt[:], wv[:, :, cb * 512:(cb + 1) * 512], (), [Bw])
                    return wt, Bw

                def p0_comp(cb, ld):
                    wt, Bw = ld
                    for j in range(4):
                        ps, Bp = pss.next()
                        for kc in range(KC):
                            MM(ps[:], wt[:, kc, j * 128:(j + 1) * 128], scT[:, kc * 2:(kc + 1) * 2],
                               kc == 0, kc == KC - 1, [Bw, B_c], [Bp])
                        ch = cb * 4 + j
                        TS('dve', modT[:, ch, :], ps[:], bmT[:, ch:ch + 1], None, ALU.add, None, [Bp, B_bm], [B_mod])
                pipeline(list(range(24)), p0_load, p0_comp)
                pr = pst(st, "pr", [96, 2, 128], F32)
                rows = sbt(st, "rows", [96, 2, 128], F32)
                B_pr, B_rows = Buf(), Buf()
                mflat = sbt(st, "mflat", [128, 2, 96], F32)
                B_mf = Buf()
                for j in range(2):
                    CP('dve', mflat[:, j, :], modT[:, :, j], [B_mod], [B_mf])
                for j in range(2):
                    TR(pr[:, j, :], mflat[:, j, :], identf[:], [B_mf, B_id], [B_pr])
                CP('dve', rows[:], pr[:], [B_pr], [B_rows])
                for j in range(2):
                    DMA('sp', MODROW[l, j].rearrange("(c p) -> c p", p=128), rows[:, j, :], [B_rows], ())
                n1 = sbt(st, "n1", [128, KC], F32)
                B_n1 = Buf()
                DMA('sp', n1[:], norm1T[l], (), [B_n1])
                for j in range(2):
                    TS('dve', A1[:, :, j], modT[:, 16:32, j], 1.0, None, ALU.add, None, [B_mod], [B_mod])
                    TT('dve', A1[:, :, j], A1[:, :, j], n1[:], ALU.mult, [B_mod, B_n1], [B_mod])
                phase_end()
            if done(l, 'P0'):
                finished = True
                break

            with ExitStack() as st:
                xts = Ring([sbt(st, f"xt{i}", [128, D], F32) for i in range(3)])
                junk = sbt(st, "junk", [128, D], BF16)
                xns = Ring([sbt(st, f"xn{i}", [128, D], F32) for i in range(2)])
                sss = Ring([sbt(st, f"ss{i}", [128, 2], F32) for i in range(2)])
                pts = Ring([pst(st, f"pt{i}", [128, KC, 128], F32) for i in range(2)])
                hbs = Ring([sbt(st, f"hb{i}", [128, KC, 512], BF16) for i in range(2)])
                B_junk = Buf()
                items = [(t0, n, sub) for (t0, n) in blocks(False) for sub in range(n // 128)]
                state = {}

                def p1_load(it):
                    t0, n, sub = it
                    xt, Bx = xts.next()
                    DMA('sp', xt[:], src_rows(l, t0 + sub * 128, 128), (), [Bx])
                    return xt, Bx

                def p1_comp(it, ld):
                    t0, n, sub = it
                    xt, Bx = ld
                    if sub == 0:
                        state['hb'] = hbs.next()
                    hb, Bh = state['hb']
                    xn, Bn = xns.next()
                    ss, Bs = sss.next()
                    pt, Bp = pts.next()
                    j = 1 if t0 < L else 0
                    ACT(junk[:], xt[:], AF.Square, [Bx], [B_junk, Bs], accum=ss[:, 0:1])
                    ACT(ss[:, 1:2], ss[:, 0:1], AF.Sqrt, [Bs], [Bs], bias=EPS, scale=1.0 / D)
                    S.op('dve', lambda e: e.reciprocal(out=ss[:, 1:2], in_=ss[:, 1:2]), [Bs], [Bs])
                    TS('dve', xn[:], xt[:], ss[:, 1:2], None, ALU.mult, None, [Bx, Bs], [Bn])
                    for fc in range(KC):
                        TR(pt[:, fc, :], xn[:, fc * 128:(fc + 1) * 128], identf[:], [Bn, B_id], [Bp])
                    for fc in range(KC):
                        o = hb[:, fc, sub * 128:(sub + 1) * 128]
                        if fc % 2 == 0:
                            ACT(o, pt[:, fc, :], AF.Identity, [Bp, B_mod], [Bh],
                                bias=modT[:, fc, j:j + 1], scale=A1[:, fc, j:j + 1])
                        else:
                            TS('dve', o, pt[:, fc, :], A1[:, fc, j:j + 1], modT[:, fc, j:j + 1], ALU.mult, ALU.add,
                               [Bp, B_mod], [Bh])
                    if sub == n // 128 - 1:
                        DMA('sp', HT[:, :, t0:t0 + n].rearrange("c p t -> p c t"), hb[:, :, 0:n], [Bh], ())
                pipeline(items, p1_load, p1_comp, ahead=2)
                phase_end()
            if done(l, 'P1'):
                finished = True
                break

            with ExitStack() as st:
                wts = Ring([sbt(st, f"wi{i}", [128, KC, 512], BF16) for i in range(2)])
                hbs = Ring([sbt(st, f"hb{i}", [128, KC, 512], BF16) for i in range(3)])
                obs = Ring([sbt(st, f"ob{i}", [128, 4, 512], BF16) for i in range(3)])
                pss = Ring([pst(st, f"pp{i}", [128, 512], F32) for i in range(6)])
                wv = w_in[l].rearrange("(kc p) c -> p kc c", p=128)
                NG = NCH // 4
                bl = blocks(False)
                items = [(g, bi) for g in range(NG) for bi in range(len(bl))]
                wslots = {}

                def p2_wload(g):
                    wt, Bw = wts.next()
                    CDMA(wt[:], wv[:, :, g * 512:(g + 1) * 512], [Bw])
                    wslots[g] = (wt, Bw)

                def p2_load(it):
                    g, bi = it
                    t0, n = bl[bi]
                    hb, Bh = hbs.next()
                    DMA('sp', hb[:, :, 0:n], HT[:, :, t0:t0 + n].rearrange("c p t -> p c t"), (), [Bh])
                    return hb, Bh

                def p2_comp(it, ld):
                    g, bi = it
                    t0, n = bl[bi]
                    hb, Bh = ld
                    if bi == 0:
                        if g == 0:
                            p2_wload(0)
                        if g + 1 < NG:
                            p2_wload(g + 1)
                    wt, Bw = wslots[g]
                    is_v = (CH_V <= g * 4 < CH_V + 8)
                    ob, Bo = obs.next()
                    if not is_v:
                        for j in range(4):
                            ch = g * 4 + j
                            ps, Bp = pss.next()
                            for kc in range(KC):
                                MM(ps[:, 0:n], wt[:, kc, j * 128:(j + 1) * 128], hb[:, kc, 0:n], kc == 0, kc == KC - 1,
                                   [Bw, Bh], [Bp])
                            if ch >= CH_G:
                                ACT(ob[:, j, 0:n], ps[:, 0:n], AF.Sigmoid, [Bp], [Bo])
                            elif CH_Q <= ch < CH_K:
                                TS('dve', ob[:, j, 0:n], ps[:, 0:n], 128.0 ** -0.5, None, ALU.mult, None, [Bp], [Bo])
                            elif j % 2 == 0:
                                CP('dve', ob[:, j, 0:n], ps[:, 0:n], [Bp], [Bo])
                            else:
                                ACT(ob[:, j, 0:n], ps[:, 0:n], AF.Copy, [Bp], [Bo])
                        DMA('sp', PT[g * 4:(g + 1) * 4, :, t0:t0 + n].rearrange("c p t -> p c t"), ob[:, :, 0:n], [Bo], ())
                    else:
                        vc0 = (g * 4 - CH_V) * 128
                        for sub in range(n // 128):
                            ps, Bp = pss.next()
                            for kc in range(KC):
                                MM(ps[:], hb[:, kc, sub * 128:(sub + 1) * 128], wt[:, kc, :], kc == 0, kc == KC - 1,
                                   [Bw, Bh], [Bp])
                            if sub % 2 == 0:
                                CP('dve', ob[:, sub, :], ps[:], [Bp], [Bo])
                            else:
                                ACT(ob[:, sub, :], ps[:], AF.Copy, [Bp], [Bo])
                        DMA('sp', VT[t0:t0 + n, vc0:vc0 + 512].rearrange("(s p) c -> p s c", p=128),
                            ob[:, 0:n // 128, :], [Bo], ())
                pipeline(items, p2_load, p2_comp, ahead=2)
                phase_end()
            if done(l, 'P2'):
                finished = True
                break

            segs = [(0, L), (L, T)]
            with ExitStack() as st:
                cw = sbt(st, "cw", [128, 8, 3], F32)
                B_cw = Buf()
                DMA('sp', cw[:], conv_wT[l].rearrange("p (c k) -> p c k", k=3), (), [B_cw])
                ins = Ring([sbt(st, f"ci{i}", [128, 3, NT], BF16) for i in range(2)])
                us = Ring([sbt(st, f"cu{i}", [128, NT], F32) for i in range(2)])
                vs = Ring([sbt(st, f"cv{i}", [128, NT], F32) for i in range(2)])
                ys = Ring([sbt(st, f"cy{i}", [128, NT], BF16) for i in range(2)])
                for c in range(8):
                    ci, Bi = ins.next()
                    u, Bu = us.next()
                    v, Bv = vs.next()
                    y, By = ys.next()
                    for k, ch in enumerate((CH_AB + c, CH_AC + c, CH_AX + c)):
                        DMA('sp', ci[:, k, :], PT[ch], (), [Bi])
                    eng = 'dve'
                    TT(eng, u[:], ci[:, 1, :], ci[:, 2, :], ALU.mult, [Bi], [Bu])
                    TS(eng, v[:], u[:], cw[:, c, 1:2], None, ALU.mult, None, [Bu, B_cw], [Bv])
                    for (s0, n) in segs:
                        STT(eng, v[:, s0 + 1:s0 + n], u[:, s0:s0 + n - 1], cw[:, c, 0:1], v[:, s0 + 1:s0 + n],
                            ALU.mult, ALU.add, [Bu, B_cw, Bv], [Bv])
                        STT(eng, v[:, s0:s0 + n - 1], u[:, s0 + 1:s0 + n], cw[:, c, 2:3], v[:, s0:s0 + n - 1],
                            ALU.mult, ALU.add, [Bu, B_cw, Bv], [Bv])
                    TT(eng, y[:], ci[:, 0, :], v[:], ALU.mult, [Bi, Bv], [By])
                    DMA('sp', CT[c], y[:], [By], ())
                phase_end()

            with ExitStack() as st:
                PADN = NT + 64
                offs = [16, 16 + L + 32]
                pws = Ring([sbt(st, f"pw{i}", [128, 2, 256], BF16) for i in range(2)])
                psc = sbt(st, "psc", [128, 8], F32)
                B_psc = Buf()
                DMA('sp', psc[:], pool_scaleT[l], (), [B_psc])
                uin = Ring([sbt(st, f"pu{i}", [128, NT], BF16) for i in range(2)])
                rcs = Ring([sbt(st, f"rc{i}", [128, NT], F32) for i in range(2)])
                mixed = sbt(st, "mixed", [128, 2, NT], BF16)
                B_mixed = Buf()
                bufa = sbt(st, "pba", [128, PADN], F32)
                bufb = sbt(st, "pbb", [128, PADN], F32)
                Ba_, Bb_ = Buf(), Buf()
                pob = Ring([sbt(st, f"po{i}", [128, 512], BF16) for i in range(3)])
                pss = Ring([pst(st, f"pq{i}", [128, 512], F32) for i in range(4)])
                for g in range(4):
                    pw, Bpw = pws.next()
                    S.dma('pool', (lambda o, i: (lambda e: e.dma_start(out=o, in_=i)))(
                        pw[:], pool_w[l, g].rearrange("(kc p) d -> p kc d", p=128)), (), [Bpw])
                    rc, Brc = rcs.next()
                    DMA('sp', rc[:], rc_tab[g:g + 1, :].to_broadcast([128, NT]), (), [Brc])
                    for cc in range(2):
                        c = 2 * g + cc
                        u, Bu = uin.next()
                        DMA('sp', u[:], PT[CH_POOL + c], (), [Bu])
                        MS('pool', bufa[:], 0.0, [Ba_])
                        for si, (s0, n) in enumerate(segs):
                            CP('pool', bufa[:, offs[si]:offs[si] + n], u[:, s0:s0 + n], [Bu], [Ba_])
                        cur, Bcur, oth, Both = bufa, Ba_, bufb, Bb_
                        sh = 0
                        for lev in range(g + 1):
                            if lev == 0:
                                lo, hi = 1, PADN
                                TT('dve', oth[:, lo:hi], cur[:, lo - 1:hi - 1], cur[:, lo:hi], ALU.add, [Bcur], [Both])
                            else:
                                h = 1 << (lev - 1)
                                lo, hi = 8, PADN - 8
                                TT('dve', oth[:, lo:hi], cur[:, lo - h:hi - h], cur[:, lo + h:hi + h], ALU.add, [Bcur], [Both])
                            cur, Bcur, oth, Both = oth, Both, cur, Bcur
                        for si, (s0, n) in enumerate(segs):
                            TT('dve', oth[:, offs[si]:offs[si] + n], cur[:, offs[si]:offs[si] + n], rc[:, s0:s0 + n], ALU.mult,
                               [Bcur, Brc], [Both])
                            TT('dve', mixed[:, cc, s0:s0 + n], oth[:, offs[si]:offs[si] + n], u[:, s0:s0 + n], ALU.subtract,
                               [Both, Bu], [B_mixed])
                    for dc in range(2):
                        c = 2 * g + dc
                        for (t0, n) in blocks(False):
                            ps, Bp = pss.next()
                            ob, Bo = pob.next()
                            for kc in range(2):
                                MM(ps[:, 0:n], pw[:, kc, dc * 128:(dc + 1) * 128], mixed[:, kc, t0:t0 + n], kc == 0, kc == 1,
                                   [Bpw, B_mixed], [Bp])
                            TS('dve', ob[:, 0:n], ps[:, 0:n], psc[:, c:c + 1], None, ALU.mult, None, [Bp, B_psc], [Bo])
                            DMA('sp', PO[c, :, t0:t0 + n], ob[:, 0:n], [Bo], ())
                phase_end()
            if done(l, 'P4'):
                finished = True
                break

            with ExitStack() as st:
                kcT = sbt(st, "kcT", [128, 8, L], BF16)
                vcx = sbt(st, "vcx", [128, 2, 1024], BF16)
                tb = sbt(st, "tb", [128, 4, 15 * 64], BF16)
                B_kc, B_vc, B_tb = Buf(), Buf(), Buf()
                DMA('sp', kcT[:], PT[CH_K:CH_K + 8, :, 0:L].rearrange("c p t -> p c t"), (), [B_kc])
                DMA('sp', vcx[:], VT[0:L, :].rearrange("(s p) c -> p s c", p=128), (), [B_vc])
                DMA('sp', tb[:], bias_tab[l].rearrange("q (h x) -> q h x", h=4), (), [B_tb])
                qts = Ring([sbt(st, f"qt{i}", [128, 8, 512], BF16) for i in range(2)])
                kws = Ring([sbt(st, f"kw{i}", [128, 8, 512], BF16) for i in range(3)])
                vws = Ring([sbt(st, f"vw{i}", [128, 4, 1024], BF16) for i in range(3)])
                aos = Ring([sbt(st, f"ao{i}", [128, 8, 512], BF16) for i in range(2)])
                sws = Ring([sbt(st, f"sw{i}", [128, 768], F32) for i in range(2)])
                pes = Ring([sbt(st, f"pe{i}", [128, 768], F32) for i in range(2)])
                pns = Ring([sbt(st, f"pn{i}", [128, 768], BF16) for i in range(2)])
                sts = Ring([sbt(st, f"st{i}", [128, 4], F32) for i in range(3)])
                ptsb = Ring([sbt(st, f"pts{i}", [128, 6, 128], BF16) for i in range(2)])
                ps_s = Ring([pst(st, f"pss{i}", [128, 512], F32) for i in range(2)])
                ps_c = Ring([pst(st, f"psc{i}", [128, 512], F32) for i in range(2)])
                ps_t = Ring([pst(st, f"pst{i}", [128, 6, 128], BF16) for i in range(2)])
                ps_o = Ring([pst(st, f"pso{i}", [128, 2, 64], F32) for i in range(2)])

                def attn_unit(q2, Bq, k2, Bk, kc2, v_fn, Bv, nwin, bias_ap, o_ap, Bo):
                    nk = nwin + L
                    sw, Bsw = sws.next()
                    pe_, Bpe = pes.next()
                    pn, Bpn = pns.next()
                    stt, Bst = sts.next()
                    pss_, Bpss = ps_s.next()
                    psc_, Bpsc = ps_c.next()
                    pst_, Bpst = ps_t.next()
                    pso_, Bpso = ps_o.next()
                    ptb, Bptb = ptsb.next()
                    for hh in range(2):
                        if nwin:
                            MM(pss_[hh * 64:(hh + 1) * 64, 0:nwin], q2(hh), k2(hh), True, True, [Bq, Bk], [Bpss])
                        MM(psc_[hh * 64:(hh + 1) * 64, 0:L], q2(hh), kc2(hh), True, True, [Bq, B_kc], [Bpsc])
                    if nwin:
                        TT('dve', sw[:, 0:nwin], pss_[:, 0:nwin], bias_ap, ALU.add, [Bpss, B_tb], [Bsw])
                    ACT(sw[:, nwin:nk], psc_[:, 0:L], AF.Copy, [Bpsc], [Bsw])
                    S.op('dve', lambda e: e.reduce_max(out=stt[:, 0:1], in_=sw[:, 0:nk], axis=AX.X), [Bsw], [Bst])
                    TS('dve', stt[:, 1:2], stt[:, 0:1], -1.0, None, ALU.mult, None, [Bst], [Bst])
                    ACT(pe_[:, 0:nk], sw[:, 0:nk], AF.Exp, [Bsw, Bst], [Bpe, Bst], bias=stt[:, 1:2], scale=1.0, accum=stt[:, 2:3])
                    S.op('dve', lambda e: e.reciprocal(out=stt[:, 3:4], in_=stt[:, 2:3]), [Bst], [Bst])
                    TS('dve', pn[:, 0:nk], pe_[:, 0:nk], stt[:, 3:4], None, ALU.mult, None, [Bpe, Bst], [Bpn])
                    nj = nk // 128
                    for j in range(nj):
                        TR(pst_[:, j, :], pn[:, j * 128:(j + 1) * 128], identb[:, :], [Bpn, B_id], [Bpst])
                    ACT(ptb[:, 0:nj, :], pst_[:, 0:nj, :], AF.Copy, [Bpst], [Bptb])
                    for hh in range(2):
                        for j in range(nj):
                            MM(pso_[:, hh, :], v_fn(hh, j), ptb[:, j, hh * 64:(hh + 1) * 64], j == 0, j == nj - 1,
                               [Bv, B_vc, Bptb], [Bpso])
                    ACT(o_ap, pso_[:], AF.Copy, [Bpso], [Bo])

                state = {'rs': -1}

                def p5_load(r):
                    res = {}
                    if r % 8 == 0:
                        qt, Bq = qts.next()
                        DMA('sp', qt[:], PT[CH_Q:CH_Q + 8, :, L + r * 64:L + (r + 8) * 64].rearrange("c p t -> p c t"), (), [Bq])
                        state['q'] = (qt, Bq)
                    rs = min(max(r - 4, 0), ROWS - 8)
                    if rs != state['rs']:
                        kw, Bk = kws.next()
                        vw, Bv = vws.next()
                        DMA('sp', kw[:], PT[CH_K:CH_K + 8, :, L + rs * 64:L + rs * 64 + 512].rearrange("c p t -> p c t"), (), [Bk])
                        DMA('sp', vw[:], VT[L + rs * 64:L + rs * 64 + 512, :].rearrange("(s p) c -> p s c", p=128), (), [Bv])
                        state['rs'] = rs
                        state['kv'] = (kw, Bk, vw, Bv)
                    return state['q'] + state['kv'] + (rs,)

                def p5_comp(r, ld):
                    qt, Bq, kw, Bk, vw, Bv, rs = ld
                    rr = r % 8
                    if rr == 0:
                        state['ao'] = aos.next()
                    ao, Bao = state['ao']
                    dr0 = rs - r + 7
                    for pr_ in range(4):
                        h0 = 2 * pr_

                        def v_fn(hh, j, h0=h0, vw=vw):
                            h = h0 + hh
                            if j < 4:
                                return vw[:, j, h * 128:(h + 1) * 128]
                            return vcx[:, j - 4, h * 128:(h + 1) * 128]
                        attn_unit(lambda hh, h0=h0: qt[:, h0 + hh, rr * 64:(rr + 1) * 64], Bq,
                                  lambda hh, h0=h0: kw[:, h0 + hh, :], Bk,
                                  lambda hh, h0=h0: kcT[:, h0 + hh, :], v_fn, Bv, 512,
                                  tb[:, pr_, dr0 * 64:(dr0 + 8) * 64], ao[:, h0:h0 + 2, rr * 64:(rr + 1) * 64], Bao)
                    if rr == 7:
                        r0 = r - 7
                        DMA('sp', AT[:, :, L + r0 * 64:L + (r0 + 8) * 64].rearrange("c p t -> p c t"), ao[:], [Bao], ())
                pipeline(list(range(ROWS)), p5_load, p5_comp, ahead=1)
                if not last:
                    qt, Bq = qts.next()
                    DMA('sp', qt[:, :, 0:L], PT[CH_Q:CH_Q + 8, :, 0:L].rearrange("c p t -> p c t"), (), [Bq])
                    ao, Bao = aos.next()
                    for qq in range(L // 64):
                        for pr_ in range(4):
                            h0 = 2 * pr_

                            def v_fn(hh, j, h0=h0):
                                h = h0 + hh
                                return vcx[:, j, h * 128:(h + 1) * 128]
                            attn_unit(lambda hh, h0=h0: qt[:, h0 + hh, qq * 64:(qq + 1) * 64], Bq, None, None,
                                      lambda hh, h0=h0: kcT[:, h0 + hh, :], v_fn, B_vc, 0, None,
                                      ao[:, h0:h0 + 2, qq * 64:(qq + 1) * 64], Bao)
                    DMA('sp', AT[:, :, 0:L].rearrange("c p t -> p c t"), ao[:, :, 0:L], [Bao], ())
                phase_end()
            if done(l, 'P5'):
                finished = True
                break

            with ExitStack() as st:
                wouts = []
                for nm, wsrc in (("wco", w_conv_out), ("wpo", w_pool_out), ("wao", w_attn_out)):
                    wt = sbt(st, nm, [128, 8, D], BF16)
                    Bw = Buf()
                    for hh in range(2):
                        CDMA(wt[:, :, hh * 1024:(hh + 1) * 1024],
                             wsrc[l].rearrange("(kc p) c -> p kc c", p=128)[:, :, hh * 1024:(hh + 1) * 1024], [Bw])
                    wouts.append((wt, Bw))
                brs = [Ring([sbt(st, f"br{b}_{i}", [128, 8, 512], BF16) for i in range(2)]) for b in range(3)]
                gts = Ring([sbt(st, f"gt{i}", [128, 3, 512], BF16) for i in range(3)])
                mbs = Ring([sbt(st, f"mb{i}", [128, KC, 512], BF16) for i in range(2)])
                tmps = Ring([sbt(st, f"tm{i}", [128, 3, 512], F32) for i in range(2)])
                pss = Ring([pst(st, f"py{i}", [128, 512], F32) for i in range(6)])

                def p6a_load(blk):
                    t0, n = blk
                    bt = []
                    for b, srcT in enumerate((CT, PO, AT)):
                        t_, B_ = brs[b].next()
                        DMA('sp', t_[:, :, 0:n], srcT[:, :, t0:t0 + n].rearrange("c p t -> p c t"), (), [B_])
                        bt.append((t_, B_))
                    return bt

                def p6a_comp(blk, bt):
                    t0, n = blk
                    mb, Bmb = mbs.next()

                    def gload(fc):
                        gt, Bg = gts.next()
                        for b in range(3):
                            DMA('sp', gt[:, b, 0:n], PT[CH_G + b * 16 + fc, :, t0:t0 + n], (), [Bg])
                        return gt, Bg

                    def gcomp(fc, ld):
                        gt, Bg = ld
                        tm, Btm = tmps.next()
                        for b in range(3):
                            ps, Bp = pss.next()
                            wt, Bw = wouts[b]
                            for kc in range(8):
                                MM(ps[:, 0:n], wt[:, kc, fc * 128:(fc + 1) * 128], bt[b][0][:, kc, 0:n], kc == 0, kc == 7,
                                   [Bw, bt[b][1]], [Bp])
                            TT('dve', tm[:, b, 0:n], ps[:, 0:n], gt[:, b, 0:n], ALU.mult, [Bp, Bg], [Btm])
                        TT('pool', tm[:, 0, 0:n], tm[:, 0, 0:n], tm[:, 1, 0:n], ALU.add, [Btm], [Btm])
                        TT('pool', mb[:, fc, 0:n], tm[:, 0, 0:n], tm[:, 2, 0:n], ALU.add, [Btm], [Bmb])
                    pipeline(list(range(KC)), gload, gcomp, ahead=2)
                    DMA('sp', MT[:, :, t0:t0 + n].rearrange("c p t -> p c t"), mb[:, :, 0:n], [Bmb], ())
                pipeline(blocks(last), p6a_load, p6a_comp, ahead=1)
                phase_end()

            with ExitStack() as st:
                wo = sbt(st, "wo", [128, KC, D], BF16)
                B_wo = Buf()
                for hh in range(4):
                    CDMA(wo[:, :, hh * 512:(hh + 1) * 512],
                         w_o[l].rearrange("(kc p) c -> p kc c", p=128)[:, :, hh * 512:(hh + 1) * 512], [B_wo])
                g1 = sbt(st, "g1", [128, 2, D], F32)
                B_g1 = Buf()
                for j in range(2):
                    DMA('sp', g1[:, j, :], MODROW[l, j:j + 1, 2 * D:3 * D].to_broadcast([128, D]), (), [B_g1])
                mbs = Ring([sbt(st, f"mb{i}", [128, KC, 512], BF16) for i in range(2)])
                xts = Ring([sbt(st, f"xt{i}", [128, D], F32) for i in range(3)])
                xos = Ring([sbt(st, f"xo{i}", [128, D], F32) for i in range(2)])
                pss = Ring([pst(st, f"pw{i}", [128, 512], F32) for i in range(4)])
                items = [(t0, n, sub) for (t0, n) in blocks(last) for sub in range(n // 128)]
                state = {}

                def p6b_load(it):
                    t0, n, sub = it
                    if sub == 0:
                        mb, Bmb = mbs.next()
                        DMA('sp', mb[:, :, 0:n], MT[:, :, t0:t0 + n].rearrange("c p t -> p c t"), (), [Bmb])
                        state['mb'] = (mb, Bmb)
                    xt, Bx = xts.next()
                    DMA('sp', xt[:], src_rows(l, t0 + sub * 128, 128), (), [Bx])
                    return state['mb'] + (xt, Bx)

                def p6b_comp(it, ld):
                    t0, n, sub = it
                    mb, Bmb, xt, Bx = ld
                    j = 1 if t0 < L else 0
                    xo, Bxo = xos.next()
                    for cb in range(4):
                        ps, Bp = pss.next()
                        for kc in range(KC):
                            MM(ps[:], mb[:, kc, sub * 128:(sub + 1) * 128], wo[:, kc, cb * 512:(cb + 1) * 512],
                               kc == 0, kc == KC - 1, [Bmb, B_wo], [Bp])
                        TT('dve', xo[:, cb * 512:(cb + 1) * 512], ps[:], g1[:, j, cb * 512:(cb + 1) * 512], ALU.mult,
                           [Bp, B_g1], [Bxo])
                        TT('pool', xo[:, cb * 512:(cb + 1) * 512], xo[:, cb * 512:(cb + 1) * 512],
                           xt[:, cb * 512:(cb + 1) * 512], ALU.add, [Bxo, Bx], [Bxo])
                    DMA('sp', XA[t0 + sub * 128:t0 + (sub + 1) * 128, :], xo[:], [Bxo], ())
                pipeline(items, p6b_load, p6b_comp, ahead=1)
                phase_end()
            if done(l, 'P6'):
                finished = True
                break

            with ExitStack() as st:
                a2 = sbt(st, "a2", [128, 2, D], F32)
                b2 = sbt(st, "b2", [128, 2, D], F32)
                n2 = sbt(st, "n2", [128, D], F32)
                B_a2 = Buf()
                DMA('sp', n2[:], norm2row[l].to_broadcast([128, D]), (), [B_a2])
                for j in range(2):
                    DMA('sp', a2[:, j, :], MODROW[l, j:j + 1, 4 * D:5 * D].to_broadcast([128, D]), (), [B_a2])
                    DMA('sp', b2[:, j, :], MODROW[l, j:j + 1, 3 * D:4 * D].to_broadcast([128, D]), (), [B_a2])
                for j in range(2):
                    TS('dve', a2[:, j, :], a2[:, j, :], 1.0, None, ALU.add, None, [B_a2], [B_a2])
                    TT('dve', a2[:, j, :], a2[:, j, :], n2[:], ALU.mult, [B_a2], [B_a2])
                wr = sbt(st, "wr", [128, KC, NE], BF16)
                B_wr = Buf()
                CDMA(wr[:], w_router[l].rearrange("(kc p) e -> p kc e", p=128), [B_wr])
                xts = Ring([sbt(st, f"xt{i}", [128, D], F32) for i in range(3)])
                junk = sbt(st, "junk", [128, D], BF16)
                B_junk = Buf()
                hfs = Ring([sbt(st, f"hf{i}", [128, D], F32) for i in range(2)])
                hbs = Ring([sbt(st, f"h2b{i}", [128, D], BF16) for i in range(2)])
                hts = Ring([sbt(st, f"h2t{i}", [128, KC, 128], BF16) for i in range(2)])
                sss = Ring([sbt(st, f"ss{i}", [128, 4], F32) for i in range(2)])
                lgs = Ring([sbt(st, f"lg{i}", [128, 2, NE], F32) for i in range(2)])
                afs = Ring([sbt(st, f"af{i}", [NE, 128], F32) for i in range(2)])
                pts = Ring([pst(st, f"pt{i}", [128, KC, 128], BF16) for i in range(2)])
                pls = Ring([pst(st, f"pl{i}", [128, NE], F32) for i in range(2)])
                pas = Ring([pst(st, f"pa{i}", [NE, 128], F32) for i in range(2)])

                def p7_load(t):
                    xt, Bx = xts.next()
                    DMA('sp', xt[:], XA[t * 128:(t + 1) * 128, :], (), [Bx])
                    return xt, Bx

                def p7_comp(t, ld):
                    xt, Bx = ld
                    j = 1 if t < 2 else 0
                    hf, Bhf = hfs.next()
                    hb, Bhb = hbs.next()
                    ht, Bht = hts.next()
                    ss, Bs = sss.next()
                    lg, Blg = lgs.next()
                    af, Baf = afs.next()
                    pt, Bp = pts.next()
                    pl, Bpl = pls.next()
                    pa, Bpa = pas.next()
                    ACT(junk[:], xt[:], AF.Square, [Bx], [B_junk, Bs], accum=ss[:, 0:1])
                    ACT(ss[:, 1:2], ss[:, 0:1], AF.Sqrt, [Bs], [Bs], bias=EPS, scale=1.0 / D)
                    S.op('dve', lambda e: e.reciprocal(out=ss[:, 1:2], in_=ss[:, 1:2]), [Bs], [Bs])
                    STT('dve', hf[:], xt[:], ss[:, 1:2], a2[:, j, :], ALU.mult, ALU.mult, [Bx, Bs, B_a2], [Bhf])
                    TT('pool', hb[:], hf[:], b2[:, j, :], ALU.add, [Bhf, B_a2], [Bhb])
                    DMA('sp', H2[t * 128:(t + 1) * 128, :], hb[:], [Bhb], ())
                    for fc in range(KC):
                        TR(pt[:, fc, :], hb[:, fc * 128:(fc + 1) * 128], identb[:], [Bhb, B_id], [Bp])
                    CP('dve', ht[:, 0:8, :], pt[:, 0:8, :], [Bp], [Bht])
                    ACT(ht[:, 8:16, :], pt[:, 8:16, :], AF.Copy, [Bp], [Bht])
                    for kc in range(KC):
                        MM(pl[:], ht[:, kc, :], wr[:, kc, :], kc == 0, kc == KC - 1, [Bht, B_wr], [Bpl])
                    S.op('dve', lambda e: e.reduce_max(out=ss[:, 2:3], in_=pl[:], axis=AX.X), [Bpl], [Bs])
                    TS('dve', ss[:, 2:3], ss[:, 2:3], -1.0, None, ALU.mult, None, [Bs], [Bs])
                    ACT(lg[:, 0, :], pl[:], AF.Exp, [Bpl, Bs], [Blg, Bs], bias=ss[:, 2:3], scale=1.0, accum=ss[:, 3:4])
                    S.op('dve', lambda e: e.reciprocal(out=ss[:, 3:4], in_=ss[:, 3:4]), [Bs], [Bs])
                    TS('dve', lg[:, 1, :], lg[:, 0, :], ss[:, 3:4], None, ALU.mult, None, [Blg, Bs], [Blg])
                    TR(pa[:], lg[:, 1, :], identf[:], [Blg, B_id], [Bpa])
                    CP('dve', af[:], pa[:], [Bpa], [Baf])
                    DMA('sp', AFFT[:, t * 128:(t + 1) * 128], af[:], [Baf], ())
                pipeline(tiles_moe, p7_load, p7_comp, ahead=2)
                phase_end()
            if done(l, 'P7'):
                finished = True
                break

            with ExitStack() as st:
                affT = sbt(st, "affT", [NE, NT], F32)
                junkm = sbt(st, "junkm", [NE, NT], F32)
                msk = sbt(st, "msk", [NE, NT], F32)
                pos = sbt(st, "pos", [NE, NT], F32)
                B_aff, B_jm, B_msk, B_pos = Buf(), Buf(), Buf(), Buf()
                s_lo = L if last else 0
                DMA('sp', affT[:, s_lo:NT], AFFT[:, s_lo:NT], (), [B_aff])
                sc = sbt(st, "sc", [NE, 8], F32)
                B_sc = Buf()
                seg_list = [(L, T, CAPX, 0.0)] if last else [(0, L, CAPC, float(CAPX)), (L, T, CAPX, 0.0)]
                for (s0, n, kk, base) in seg_list:
                    lo, hi, mid, cnt, cc, dd = [sc[:, i:i + 1] for i in range(6)]
                    MS('dve', lo, 0.0, [B_sc])
                    MS('dve', hi, 1.0, [B_sc])
                    for it in range(32):
                        TT('dve', mid, lo, hi, ALU.add, [B_sc], [B_sc])
                        TS('dve', mid, mid, 0.5, None, ALU.mult, None, [B_sc], [B_sc])
                        TS('dve', junkm[:, s0:s0 + n], affT[:, s0:s0 + n], mid, None, ALU.is_ge, ALU.add,
                           [B_aff, B_sc], [B_jm, B_sc], accum=cnt)
                        TS('dve', cc, cnt, float(kk) - 0.5, None, ALU.is_ge, None, [B_sc], [B_sc])
                        TT('dve', dd, mid, lo, ALU.subtract, [B_sc], [B_sc])
                        STT('dve', lo, dd, cc, lo, ALU.mult, ALU.add, [B_sc], [B_sc])
                        TT('dve', dd, hi, mid, ALU.subtract, [B_sc], [B_sc])
                        STT('dve', hi, dd, cc, mid, ALU.mult, ALU.add, [B_sc], [B_sc])
                    TS('dve', msk[:, s0:s0 + n], affT[:, s0:s0 + n], lo, None, ALU.is_ge, None, [B_aff, B_sc], [B_msk])
                    S.op('dve', (lambda o, i: (lambda e: e.tensor_tensor_scan(out=o, data0=i, data1=i, initial=0.0,
                                                                              op0=ALU.add, op1=ALU.max)))(
                        pos[:, s0:s0 + n], msk[:, s0:s0 + n]), [B_msk], [B_pos])
                    TS('dve', junkm[:, s0:s0 + n], pos[:, s0:s0 + n], float(kk) + 0.5, None, ALU.is_le, None, [B_pos], [B_jm])
                    TT('dve', msk[:, s0:s0 + n], msk[:, s0:s0 + n], junkm[:, s0:s0 + n], ALU.mult, [B_msk, B_jm], [B_msk])
                    TS('dve', pos[:, s0:s0 + n], pos[:, s0:s0 + n], base - 1.0 - BIG, None, ALU.add, None, [B_pos], [B_pos])
                    TT('dve', pos[:, s0:s0 + n], pos[:, s0:s0 + n], msk[:, s0:s0 + n], ALU.mult, [B_pos, B_msk], [B_pos])
                    TS('dve', pos[:, s0:s0 + n], pos[:, s0:s0 + n], BIG, None, ALU.add, None, [B_pos], [B_pos])
                    TT('dve', msk[:, s0:s0 + n], msk[:, s0:s0 + n], affT[:, s0:s0 + n], ALU.mult, [B_msk, B_aff], [B_msk])
                ptk = Ring([pst(st, f"ptk{i}", [128, 2, NE], F32) for i in range(2)])
                idxf = sbt(st, "idxf", [128, NTILE, NE], F32)
                B_if = Buf()
                for t in tiles_moe:
                    p_, Bp_ = ptk.next()
                    TR(p_[:, 0, :], pos[:, t * 128:(t + 1) * 128], identf[0:NE, 0:NE], [B_pos, B_id], [Bp_])
                    TR(p_[:, 1, :], msk[:, t * 128:(t + 1) * 128], identf[0:NE, 0:NE], [B_msk, B_id], [Bp_])
                    CP('dve', idxf[:, t, :], p_[:, 0, :], [Bp_], [B_if])
                    CP('dve', gmt[:, t, :], p_[:, 1, :], [Bp_], [B_rt])
                CP('dve', idxi[:], idxf[:], [B_if], [B_rt])
                if 'IDXD' in dbg:
                    DMA('sp', IDXD.rearrange("(t p) e -> p t e", p=128), idxf[:], [B_if], ())
                    DMA('sp', AFFD.rearrange("(t p) e -> p t e", p=128), gmt[:], [B_rt], ())
                phase_end()
            if done(l, 'P8'):
                finished = True
                break

            with ExitStack() as st:
                hbs = Ring([sbt(st, f"h2b{i}", [128, D], BF16) for i in range(3)])

                def p9_load(t):
                    hb, Bhb = hbs.next()
                    DMA('sp', hb[:], H2[t * 128:(t + 1) * 128, :], (), [Bhb])
                    return hb, Bhb

                def p9_comp(t, ld):
                    hb, Bhb = ld
                    for e_ in range(NE):
                        S.dma('pool', (lambda o, ia, i: (lambda e: e.indirect_dma_start(
                            out=o, out_offset=bass.IndirectOffsetOnAxis(ap=ia, axis=0), in_=i, in_offset=None,
                            bounds_check=S.getreg(e, nslot - 1), oob_is_err=False)))(XS[e_], idxi[:, t, e_:e_ + 1], hb[:, :]),
                            [Bhb, B_rt], ())
                pipeline(tiles_moe, p9_load, p9_comp, ahead=2)
                phase_end()
            if done(l, 'P9'):
                finished = True
                break

            with ExitStack() as st:
                nst = (nslot + 127) // 128
                xrs = Ring([sbt(st, f"xr{i}", [128, D], BF16) for i in range(3)])
                xsT = Ring([sbt(st, f"xsT{i}", [128, KC, NSLOT], BF16) for i in range(2)])
                hT = Ring([sbt(st, f"hT{i}", [128, KC, NSLOT], BF16) for i in range(1)])
                wgs = Ring([sbt(st, f"wg{i}", [128, KC, 512], BF16) for i in range(2)])
                wus = Ring([sbt(st, f"wu{i}", [128, KC, 512], BF16) for i in range(2)])
                wds = Ring([sbt(st, f"wd{i}", [128, KC, 512], BF16) for i in range(2)])
                sgs = Ring([sbt(st, f"sg{i}", [128, NSLOT], F32) for i in range(2)])
                yos = Ring([sbt(st, f"yo{i}", [128, 512], F32) for i in range(3)])
                ptr = Ring([pst(st, f"ptr{i}", [128, 4, 128], BF16) for i in range(2)])
                pg = Ring([pst(st, f"pg{i}", [128, 2, 512], F32) for i in range(1)])
                pu = Ring([pst(st, f"pu{i}", [128, 2, 512], F32) for i in range(1)])
                pd = Ring([pst(st, f"pd{i}", [128, 512], F32) for i in range(2)])
                nsp = [(0, min(512, nslot))] + ([(512, nslot - 512)] if nslot > 512 else [])
                g2e = sbt(st, "g2e", [128, 2, D], F32)
                B_g2e = Buf()
                for j in range(2):
                    DMA('sp', g2e[:, j, :], MODROW[l, j:j + 1, 5 * D:6 * D].to_broadcast([128, D]), (), [B_g2e])
                jobs = []
                for e_ in range(NE):
                    for fb in range(4):
                        jobs.append((e_, 'gu', fb))
                    for db in range(4):
                        jobs.append((e_, 'd', db))
                state = {}

                def p10_load(job):
                    e_, kind, blk = job
                    if kind == 'gu':
                        wg, Bwg = wgs.next()
                        wu, Bwu = wus.next()
                        CDMA(wg[:], w_e_gate[l, e_].rearrange("(kc p) f -> p kc f", p=128)[:, :, blk * 512:(blk + 1) * 512], [Bwg])
                        CDMA(wu[:], w_e_up[l, e_].rearrange("(kc p) f -> p kc f", p=128)[:, :, blk * 512:(blk + 1) * 512], [Bwu])
                        return (wg, Bwg, wu, Bwu)
                    wd, Bwd = wds.next()
                    CDMA(wd[:], w_e_down[l, e_].rearrange("(kc p) d -> p kc d", p=128)[:, :, blk * 512:(blk + 1) * 512], [Bwd])
                    return (wd, Bwd)

                def p10_comp(job, ld):
                    e_, kind, blk = job
                    if kind == 'gu' and blk == 0:
                        xT, BxT = xsT.next()
                        for s_ in range(nst):
                            n = min(128, nslot - s_ * 128)
                            xr, Bxr = xrs.next()
                            DMA('sp', xr[0:n, :], XS[e_][s_ * 128:s_ * 128 + n, :], (), [Bxr])
                            for q4 in range(4):
                                p_, Bp_ = ptr.next()
                                for jj in range(4):
                                    fc = q4 * 4 + jj
                                    TR(p_[:, jj, 0:n], xr[0:n, fc * 128:(fc + 1) * 128], identb[0:n, 0:n], [Bxr, B_id], [Bp_])
                                if q4 % 2 == 0:
                                    CP('dve', xT[:, q4 * 4:(q4 + 1) * 4, s_ * 128:s_ * 128 + n], p_[:, :, 0:n], [Bp_], [BxT])
                                else:
                                    ACT(xT[:, q4 * 4:(q4 + 1) * 4, s_ * 128:s_ * 128 + n], p_[:, :, 0:n], AF.Copy, [Bp_], [BxT])
                        state['xT'] = (xT, BxT)
                        state['h'] = hT.next()
                    xT, BxT = state['xT']
                    h_, Bh_ = state['h']
                    if kind == 'gu':
                        wg, Bwg, wu, Bwu = ld
                        for jj in range(4):
                            fch = blk * 4 + jj
                            g_, Bg_ = pg.next()
                            u_, Bu_ = pu.next()
                            sg, Bsg = sgs.next()
                            for (c0, cn) in nsp:
                                bi = 0 if c0 == 0 else 1
                                for kc in range(KC):
                                    MM(g_[:, bi, 0:cn], wg[:, kc, jj * 128:(jj + 1) * 128], xT[:, kc, c0:c0 + cn],
                                       kc == 0, kc == KC - 1, [Bwg, BxT], [Bg_])
                                for kc in range(KC):
                                    MM(u_[:, bi, 0:cn], wu[:, kc, jj * 128:(jj + 1) * 128], xT[:, kc, c0:c0 + cn],
                                       kc == 0, kc == KC - 1, [Bwu, BxT], [Bu_])
                                ACT(sg[:, c0:c0 + cn], g_[:, bi, 0:cn], AF.Silu, [Bg_], [Bsg])
                                TT('dve', h_[:, fch, c0:c0 + cn], sg[:, c0:c0 + cn], u_[:, bi, 0:cn], ALU.mult, [Bsg, Bu_], [Bh_])
                    else:
                        wd, Bwd = ld
                        for s_ in range(nst):
                            n = min(128, nslot - s_ * 128)
                            p_, Bp_ = pd.next()
                            yo, Byo = yos.next()
                            for fc in range(KC):
                                MM(p_[0:n, :], h_[:, fc, s_ * 128:s_ * 128 + n], wd[:, fc, :], fc == 0, fc == KC - 1,
                                   [Bh_, Bwd], [Bp_])
                            jc = 1 if s_ * 128 >= CAPX else 0
                            TT('dve', yo[0:n, :], p_[0:n, :], g2e[0:n, jc, blk * 512:(blk + 1) * 512], ALU.mult,
                               [Bp_, B_g2e], [Byo])
                            DMA('sp', YS[e_][s_ * 128:s_ * 128 + n, blk * 512:(blk + 1) * 512], yo[0:n, :], [Byo], ())
                pipeline(jobs, p10_load, p10_comp, ahead=1)
                phase_end()
            if done(l, 'P10'):
                finished = True
                break

            with ExitStack() as st:
                g2 = sbt(st, "g2", [128, 2, D], F32)
                B_g2 = Buf()
                for j in range(2):
                    DMA('sp', g2[:, j, :], MODROW[l, j:j + 1, 5 * D:6 * D].to_broadcast([128, D]), (), [B_g2])
                fn = sbt(st, "fn", [128, D], F32)
                DMA('sp', fn[:], fnorm_row.to_broadcast([128, D]), (), [B_g2])
                gbs = Ring([sbt(st, f"gb{i}", [128, D], F32) for i in range(4)])
                for (gb, Bgb) in gbs.t:
                    MS('dve', gb[:], 0.0, [Bgb])
                accs = Ring([sbt(st, f"acc{i}", [128, D], F32) for i in range(2)])
                xts = Ring([sbt(st, f"xt{i}", [128, D], F32) for i in range(3)])
                junk = sbt(st, "junk", [128, D], BF16)
                B_junk = Buf()
                sss = Ring([sbt(st, f"ss{i}", [128, 2], F32) for i in range(2)])

                def p11_load(t):
                    xt, Bx = xts.next()
                    DMA('sp', xt[:], XA[t * 128:(t + 1) * 128, :], (), [Bx])
                    return xt, Bx

                def p11_comp(t, ld):
                    xt, Bx = ld
                    j = 1 if t < 2 else 0
                    acc, Bacc = accs.next()
                    for e_ in range(NE):
                        gb, Bgb = gbs.next()
                        S.dma('pool', (lambda o, ia, i: (lambda e: e.indirect_dma_start(
                            out=o, out_offset=None, in_=i, in_offset=bass.IndirectOffsetOnAxis(ap=ia, axis=0),
                            bounds_check=S.getreg(e, nslot - 1), oob_is_err=False)))(gb[:, :], idxi[:, t, e_:e_ + 1], YS[e_]),
                            [B_rt], [Bgb])
                        if e_ == 0:
                            STT('dve', acc[:], gb[:], gmt[:, t, e_:e_ + 1], xt[:], ALU.mult, ALU.add, [Bgb, B_rt, Bx], [Bacc])
                        else:
                            STT('dve', acc[:], gb[:], gmt[:, t, e_:e_ + 1], acc[:], ALU.mult, ALU.add, [Bgb, B_rt, Bacc], [Bacc])
                    if not last:
                        DMA('sp', XB[t * 128:(t + 1) * 128, :], acc[:], [Bacc], ())
                    else:
                        ss, Bs = sss.next()
                        ACT(junk[:], acc[:], AF.Square, [Bacc], [B_junk, Bs], accum=ss[:, 0:1])
                        ACT(ss[:, 1:2], ss[:, 0:1], AF.Sqrt, [Bs], [Bs], bias=EPS, scale=1.0 / D)
                        S.op('dve', lambda e: e.reciprocal(out=ss[:, 1:2], in_=ss[:, 1:2]), [Bs], [Bs])
                        STT('dve', xt[:], acc[:], ss[:, 1:2], fn[:], ALU.mult, ALU.mult, [Bacc, Bs, B_g2, Bx], [Bx])
                        DMA('sp', out[(t - 2) * 128:(t - 1) * 128, :], xt[:], [Bx], ())
                pipeline(tiles_moe, p11_load, p11_comp, ahead=2)
                phase_end()
            if done(l, 'P11'):
                finished = True
                break
        S.barrier()
        S.flush()
        print("build: instructions", S.ninst, "max dma sem", max(S.dcnt), "sems", len(S.handles))
        assert max(S.dcnt) < 2040 and len(S.handles) <= 100
    nc._declared_inputs = declared
    return nc


def prep_inputs(inp, s):
    f = np.float32
    c2 = np.stack([inp['c'][s], inp['c_ctx']], axis=-1)
    cT = np.ascontiguousarray(c2.reshape(KC, 128, 2).transpose(1, 0, 2).reshape(128, 2 * KC)).astype(f)
    m = {
        "x": np.ascontiguousarray(inp['x'][s]),
        "ctx": np.ascontiguousarray(inp['ctx'][s]),
        "cT": cT,
    }
    return m


def shared_inputs(inp):
    f = np.float32
    import ml_dtypes
    sh = {}
    sh["w_mod"] = inp['w_mod']
    sh["b_modT"] = np.ascontiguousarray(inp['b_mod'].reshape(DEPTH, 96, 128).transpose(0, 2, 1))
    sh["norm1T"] = np.ascontiguousarray(inp['norm1'].reshape(DEPTH, KC, 128).transpose(0, 2, 1))
    sh["norm2"] = np.ascontiguousarray(inp['norm2'].reshape(DEPTH, 1, D))
    sh["w_in"] = inp['w_in']
    sh["conv_wT"] = np.ascontiguousarray(inp['conv_w'].reshape(DEPTH, 3, 8, 128).transpose(0, 3, 2, 1).reshape(DEPTH, 128, 24))
    sh["pool_w"] = inp['pool_w']
    sh["pool_scaleT"] = np.ascontiguousarray(inp['pool_scale'].reshape(DEPTH, 8, 128).transpose(0, 2, 1))
    rc = np.zeros((4, NT), f)
    for g, w in enumerate((2, 4, 8, 16)):
        for (s0, n) in ((0, L), (L, T)):
            t = np.arange(n)
            lo = np.clip(t - w // 2, 0, n)
            hi = np.clip(t - w // 2 + w, 0, n)
            rc[g, s0:s0 + n] = (1.0 / (hi - lo)).astype(f)
    sh["rc_tab"] = rc
    col = np.arange(GW)
    cs = np.clip(col - 8, 0, GW - 16)
    kcol = np.arange(GW)
    inwin = (kcol[None, :] >= cs[:, None]) & (kcol[None, :] < cs[:, None] + 16)
    dc = np.clip(kcol[None, :] - col[:, None] + 15, 0, 30)
    rpb = inp['rpb']
    tabs = rpb[:, :, :, dc]
    tabs = np.where(inwin[None, None, None], tabs, f(NEG))
    tabs = tabs.reshape(DEPTH, 4, 2, 15, GW, GW)
    tabs = np.ascontiguousarray(tabs.transpose(0, 2, 4, 1, 3, 5)).reshape(DEPTH, 2 * GW, 4 * 15 * GW)
    sh["bias_tab"] = tabs.astype(ml_dtypes.bfloat16)
    for k in ("w_conv_out", "w_pool_out", "w_attn_out", "w_o", "w_router", "w_e_gate", "w_e_up", "w_e_down"):
        sh[k] = inp[k]
    sh["final_norm"] = np.ascontiguousarray(inp['final_norm'].reshape(1, D))
    return sh


def kernel(**inputs):
    inp = {k: np.asarray(v) for k, v in inputs.items()}
    nc = build({})
    sh = shared_inputs(inp)
    in_maps = []
    for core in range(NCORES):
        m = dict(sh)
        m.update(prep_inputs(inp, core % 4))
        in_maps.append(m)
    res = run_bass_kernel_spmd(nc, in_maps, core_ids=list(range(NCORES)))
    outs = [np.asarray(res.results[i]["out"]).reshape(T, D) for i in range(4)]
    return np.stack(outs, axis=0).astype(np.float32)
```
